# Optimizing a Trainium2 kernel written in Bass

```python
import math
import jax, jax.numpy as jnp
from jax import lax
import numpy as np

D_MODEL = 2048
BATCH = 8
SEQ = 4096
DEPTH = 2

F32 = jnp.float32
NORM_EPS = 1e-6
PLE_DIM = 256
N_BRANCH = 4
BRANCH_WIDTH = D_MODEL // 2
CONV_WIDTH = 4
SSM_HEAD_DIM = 64
SSM_HEADS = BRANCH_WIDTH // SSM_HEAD_DIM
SSM_GROUPS = 4
SSM_STATE = 128
SSM_CHUNK = 128
SSM_CONV_DIM = BRANCH_WIDTH + 2 * SSM_GROUPS * SSM_STATE
HGRN_HEAD_DIM = 128
HGRN_HEADS = BRANCH_WIDTH // HGRN_HEAD_DIM
HGRN_CHUNK = 64
MLSTM_HEADS = 4
MLSTM_QK_DIM = BRANCH_WIDTH // (2 * MLSTM_HEADS)
MLSTM_V_DIM = BRANCH_WIDTH // MLSTM_HEADS
MLSTM_CHUNK = 64
LRU_BLOCKS = 8
LRU_BLOCK_DIM = BRANCH_WIDTH // LRU_BLOCKS
LRU_C = 8.0
D_FF = 4 * D_MODEL
IN_WIDTHS = (
    BRANCH_WIDTH, SSM_CONV_DIM, SSM_HEADS,
    BRANCH_WIDTH, BRANCH_WIDTH, BRANCH_WIDTH, BRANCH_WIDTH,
    MLSTM_HEADS * MLSTM_QK_DIM, MLSTM_HEADS * MLSTM_QK_DIM,
    BRANCH_WIDTH, BRANCH_WIDTH, MLSTM_HEADS, MLSTM_HEADS,
    BRANCH_WIDTH, BRANCH_WIDTH,
    N_BRANCH * D_MODEL,
)
D_IN = sum(IN_WIDTHS)

kernel_name = 'hybrid_gated_ssd_hgrn2_mlstm_rglru'


def rmsnorm(x, w):
    xf = x.astype(F32)
    y = xf * lax.rsqrt(jnp.mean(xf * xf, axis=-1, keepdims=True) + NORM_EPS)
    return (y * w.astype(F32)).astype(x.dtype)


def causal_dwconv(x, w, b):
    width, ch = w.shape
    y = lax.conv_general_dilated(x, w[:, None, :].astype(x.dtype), window_strides=(1,),
                                 padding=[(width - 1, 0)], dimension_numbers=('NWC', 'WIO', 'NWC'),
                                 feature_group_count=ch)
    return y + b.astype(x.dtype)


def segsum(a):
    t = a.shape[-1]
    cs = jnp.cumsum(a, axis=-1)
    mask = jnp.tril(jnp.ones((t, t), dtype=bool))
    return jnp.where(mask, cs[..., :, None] - cs[..., None, :], -jnp.inf)


def to_chunks(t, chunk):
    b, l, h = t.shape[:3]
    t = t.reshape(b, l // chunk, chunk, h, *t.shape[3:])
    return jnp.moveaxis(t, (1, 3), (0, 2))


def from_chunks(t):
    nc, b, h, c = t.shape[:4]
    t = jnp.moveaxis(t, (0, 2), (1, 3))
    return t.reshape(b, nc * c, h, *t.shape[4:])


def ssd_chunked(xs, dt, a, bm, cm):
    b, l, h, p = xs.shape
    g, n = bm.shape[2], bm.shape[3]
    r = h // g
    c = l // SSM_CHUNK
    x_dt = (xs * dt[..., None]).reshape(b, c, SSM_CHUNK, g, r, p)
    a_dt = jnp.transpose((dt * a).reshape(b, c, SSM_CHUNK, g, r), (0, 3, 4, 1, 2))
    bc = bm.reshape(b, c, SSM_CHUNK, g, n)
    cc = cm.reshape(b, c, SSM_CHUNK, g, n)
    a_cs = jnp.cumsum(a_dt, axis=-1)
    y_diag = jnp.einsum('bclgn,bcsgn,bgrcls,bcsgrp->bclgrp', cc, bc, jnp.exp(segsum(a_dt)), x_dt)
    decay_states = jnp.exp(a_cs[..., -1:] - a_cs)
    states = jnp.einsum('bcsgn,bgrcs,bcsgrp->bcgrpn', bc, decay_states, x_dt)
    states = jnp.concatenate([jnp.zeros_like(states[:, :1]), states], axis=1)
    chunk_decay = jnp.exp(segsum(jnp.pad(a_cs[..., -1], [(0, 0)] * 3 + [(1, 0)])))
    states = jnp.einsum('bgrzc,bcgrpn->bzgrpn', chunk_decay, states)[:, :-1]
    y_off = jnp.einsum('bclgn,bcgrpn,bgrcl->bclgrp', cc, states, jnp.exp(a_cs))
    return (y_diag + y_off).reshape(b, l, h, p)


def mamba2_mixer(z, xbc, dt_pre, conv_w, conv_b, dt_bias, a_log, d_skip, norm_w):
    b, l, _ = z.shape
    xbc = jax.nn.silu(causal_dwconv(xbc, conv_w, conv_b)).astype(F32)
    xs, bm, cm = jnp.split(xbc, [BRANCH_WIDTH, BRANCH_WIDTH + SSM_GROUPS * SSM_STATE], axis=-1)
    xs = xs.reshape(b, l, SSM_HEADS, SSM_HEAD_DIM)
    bm = bm.reshape(b, l, SSM_GROUPS, SSM_STATE)
    cm = cm.reshape(b, l, SSM_GROUPS, SSM_STATE)
    dt = jax.nn.softplus(dt_pre.astype(F32) + dt_bias.astype(F32))
    a = -jnp.exp(a_log.astype(F32))
    y = ssd_chunked(xs, dt, a, bm, cm) + d_skip.astype(F32)[:, None] * xs
    y = y.reshape(b, l, BRANCH_WIDTH) * jax.nn.silu(z.astype(F32))
    y = rmsnorm(y.reshape(b, l, SSM_GROUPS, -1), norm_w.reshape(SSM_GROUPS, -1))
    return y.reshape(b, l, BRANCH_WIDTH)


def gla_chunk_step(s, inp):
    q, k, v, g = inp
    c = q.shape[2]
    gc = jnp.cumsum(g, axis=2)
    causal = jnp.tril(jnp.ones((c, c), dtype=bool))
    decay = jnp.exp(jnp.where(causal[:, :, None], gc[:, :, :, None, :] - gc[:, :, None, :, :], -jnp.inf))
    attn = jnp.einsum('bhid,bhjd,bhijd->bhij', q, k, decay)
    o = attn @ v + jnp.einsum('bhid,bhde->bhie', q * jnp.exp(gc), s)
    g_last = gc[:, :, -1]
    s = jnp.exp(g_last)[..., None] * s + jnp.einsum('bhjd,bhje->bhde', k * jnp.exp(g_last[:, :, None] - gc), v)
    return s, o


def hgrn2_mixer(q_pre, f_pre, i_in, g_pre, lb, norm_w):
    b, l, _ = q_pre.shape
    shp = (b, l, HGRN_HEADS, HGRN_HEAD_DIM)
    f_pre = f_pre.astype(F32)
    lb = lb.astype(F32)
    q = jax.nn.silu(q_pre.astype(F32)) * HGRN_HEAD_DIM ** -0.5
    log_f = jnp.logaddexp(jnp.log(lb), jnp.log1p(-lb) + jax.nn.log_sigmoid(f_pre))
    k = (1.0 - lb) * jax.nn.sigmoid(-f_pre)
    xs = tuple(to_chunks(t.reshape(shp), HGRN_CHUNK) for t in (q, k, i_in.astype(F32), log_f))
    s0 = jnp.zeros((b, HGRN_HEADS, HGRN_HEAD_DIM, HGRN_HEAD_DIM), F32)
    _, o = lax.scan(gla_chunk_step, s0, xs)
    o = rmsnorm(from_chunks(o), norm_w.reshape(HGRN_HEADS, HGRN_HEAD_DIM)).reshape(b, l, BRANCH_WIDTH)
    return o * jax.nn.silu(g_pre.astype(F32))


def mlstm_chunk_step(carry, inp):
    c_st, n_st, m_st = carry
    q, k, v, ig, lf = inp
    c = q.shape[2]
    bcum = jnp.cumsum(lf, axis=-1)
    causal = jnp.tril(jnp.ones((c, c), dtype=bool))
    dlog = jnp.where(causal, bcum[..., :, None] - bcum[..., None, :] + ig[..., None, :], -jnp.inf)
    inter_log = bcum + m_st[..., None]
    m = jnp.maximum(inter_log, jnp.max(dlog, axis=-1))
    w_intra = jnp.exp(dlog - m[..., None])
    w_inter = jnp.exp(inter_log - m)
    s = jnp.einsum('bhid,bhjd->bhij', q, k) * w_intra
    num = s @ v + w_inter[..., None] * jnp.einsum('bhid,bhde->bhie', q, c_st)
    den = jnp.sum(s, axis=-1) + w_inter * jnp.einsum('bhid,bhd->bhi', q, n_st)
    h = num / jnp.maximum(jnp.abs(den), jnp.exp(-m))[..., None]
    b_last = bcum[..., -1]
    log_w = b_last[..., None] - bcum + ig
    m_new = jnp.maximum(b_last + m_st, jnp.max(log_w, axis=-1))
    wk = jnp.exp(log_w - m_new[..., None])
    decay = jnp.exp(b_last + m_st - m_new)
    c_st = decay[..., None, None] * c_st + jnp.einsum('bhj,bhjd,bhje->bhde', wk, k, v)
    n_st = decay[..., None] * n_st + jnp.einsum('bhj,bhjd->bhd', wk, k)
    return (c_st, n_st, m_new), h


def mlstm_mixer(q_pre, k_pre, v_in, o_pre, i_pre, f_pre, i_bias, f_bias, norm_w):
    b, l, _ = q_pre.shape
    q = q_pre.astype(F32).reshape(b, l, MLSTM_HEADS, MLSTM_QK_DIM) * MLSTM_QK_DIM ** -0.5
    k = k_pre.astype(F32).reshape(b, l, MLSTM_HEADS, MLSTM_QK_DIM)
    v = v_in.astype(F32).reshape(b, l, MLSTM_HEADS, MLSTM_V_DIM)
    ig = i_pre.astype(F32) + i_bias.astype(F32)
    lf = jax.nn.log_sigmoid(f_pre.astype(F32) + f_bias.astype(F32))
    xs = tuple(to_chunks(t, MLSTM_CHUNK) for t in (q, k, v, ig, lf))
    carry0 = (jnp.zeros((b, MLSTM_HEADS, MLSTM_QK_DIM, MLSTM_V_DIM), F32),
              jnp.zeros((b, MLSTM_HEADS, MLSTM_QK_DIM), F32),
              jnp.zeros((b, MLSTM_HEADS), F32))
    _, h = lax.scan(mlstm_chunk_step, carry0, xs)
    h = rmsnorm(from_chunks(h), norm_w.reshape(MLSTM_HEADS, MLSTM_V_DIM)).reshape(b, l, BRANCH_WIDTH)
    return h * jax.nn.sigmoid(o_pre.astype(F32))


def lru_combine(left, right):
    a1, b1 = left
    a2, b2 = right
    return a1 * a2, a2 * b1 + b2


def rglru_mixer(x_in, gate_in, conv_w, conv_b, wa, ba, wi, bi, a_param):
    b, l, _ = x_in.shape
    xc = causal_dwconv(x_in, conv_w, conv_b).astype(F32)
    xb = xc.reshape(b, l, LRU_BLOCKS, LRU_BLOCK_DIM)
    r = jax.nn.sigmoid(jnp.einsum('blnc,ncd->blnd', xb, wa.astype(F32)).reshape(b, l, BRANCH_WIDTH) + ba.astype(F32))
    i = jax.nn.sigmoid(jnp.einsum('blnc,ncd->blnd', xb, wi.astype(F32)).reshape(b, l, BRANCH_WIDTH) + bi.astype(F32))
    log_a = -LRU_C * r * jax.nn.softplus(-a_param.astype(F32))
    u = xc * i * jnp.sqrt(-jnp.expm1(2.0 * log_a))
    _, h = lax.associative_scan(lru_combine, (jnp.exp(log_a), u), axis=1)
    return h * jax.nn.gelu(gate_in.astype(F32))


def setup_inputs(seed: int = 0) -> dict:
    key = jax.random.key(seed)
    k = jax.random.split(key, 32)
    nrm = lambda kk, shape, scale: scale * jax.random.normal(kk, shape, F32)
    gain = lambda kk, shape: 1.0 + 0.02 * jax.random.normal(kk, shape, F32)
    dt = jnp.exp(jax.random.uniform(k[6], (DEPTH, SSM_HEADS), F32, math.log(1e-3), math.log(1e-1)))
    u = jax.random.uniform(k[21], (DEPTH, BRANCH_WIDTH), F32, 0.9, 0.999)
    s = u ** (1.0 / LRU_C)
    return {
        'x': nrm(k[0], (BATCH, SEQ, D_MODEL), 1.0),
        'p': nrm(k[1], (DEPTH, BATCH, SEQ, PLE_DIM), 1.0),
        'mix_norm': gain(k[2], (DEPTH, D_MODEL)),
        'w_in': nrm(k[3], (DEPTH, D_MODEL, D_IN), D_MODEL ** -0.5),
        'ssm_conv_w': nrm(k[4], (DEPTH, CONV_WIDTH, SSM_CONV_DIM), CONV_WIDTH ** -0.5),
        'ssm_conv_b': nrm(k[5], (DEPTH, SSM_CONV_DIM), 0.02),
        'ssm_dt_bias': dt + jnp.log(-jnp.expm1(-dt)),
        'ssm_a_log': jnp.log(jax.random.uniform(k[7], (DEPTH, SSM_HEADS), F32, 1.0, 16.0)),
        'ssm_d': gain(k[8], (DEPTH, SSM_HEADS)),
        'ssm_norm': gain(k[9], (DEPTH, BRANCH_WIDTH)),
        'hgrn_lb_logits': nrm(k[10], (DEPTH, BRANCH_WIDTH), 0.1),
        'hgrn_norm': gain(k[11], (DEPTH, BRANCH_WIDTH)),
        'mlstm_i_bias': -1.0 + nrm(k[12], (DEPTH, MLSTM_HEADS), 0.1),
        'mlstm_f_bias': jnp.linspace(3.0, 6.0, MLSTM_HEADS, dtype=F32)[None, :] + nrm(k[13], (DEPTH, MLSTM_HEADS), 0.1),
        'mlstm_norm': gain(k[14], (DEPTH, BRANCH_WIDTH)),
        'lru_conv_w': nrm(k[15], (DEPTH, CONV_WIDTH, BRANCH_WIDTH), CONV_WIDTH ** -0.5),
        'lru_conv_b': nrm(k[16], (DEPTH, BRANCH_WIDTH), 0.02),
        'lru_wa': nrm(k[17], (DEPTH, LRU_BLOCKS, LRU_BLOCK_DIM, LRU_BLOCK_DIM), LRU_BLOCK_DIM ** -0.5),
        'lru_ba': nrm(k[18], (DEPTH, BRANCH_WIDTH), 0.02),
        'lru_wi': nrm(k[19], (DEPTH, LRU_BLOCKS, LRU_BLOCK_DIM, LRU_BLOCK_DIM), LRU_BLOCK_DIM ** -0.5),
        'lru_bi': nrm(k[20], (DEPTH, BRANCH_WIDTH), 0.02),
        'lru_a_param': jnp.log(s) - jnp.log1p(-s),
        'w_branch': nrm(k[22], (DEPTH, N_BRANCH, BRANCH_WIDTH, D_MODEL), BRANCH_WIDTH ** -0.5),
        'w_out': nrm(k[23], (DEPTH, D_MODEL, D_MODEL), D_MODEL ** -0.5),
        'mlp_norm': gain(k[24], (DEPTH, D_MODEL)),
        'w_up': nrm(k[25], (DEPTH, D_MODEL, D_FF), D_MODEL ** -0.5),
        'w_down': nrm(k[26], (DEPTH, D_FF, D_MODEL), D_FF ** -0.5),
        'ple_norm': gain(k[27], (DEPTH, D_MODEL)),
        'w_ple': nrm(k[28], (DEPTH, PLE_DIM, D_MODEL), PLE_DIM ** -0.5),
        'w_ple_gate': nrm(k[29], (DEPTH, D_MODEL, D_MODEL), D_MODEL ** -0.5),
        'final_norm': gain(k[30], (D_MODEL,)),
    }


def reference(x, p, mix_norm, w_in, ssm_conv_w, ssm_conv_b, ssm_dt_bias, ssm_a_log, ssm_d, ssm_norm,
              hgrn_lb_logits, hgrn_norm, mlstm_i_bias, mlstm_f_bias, mlstm_norm,
              lru_conv_w, lru_conv_b, lru_wa, lru_ba, lru_wi, lru_bi, lru_a_param,
              w_branch, w_out, mlp_norm, w_up, w_down, ple_norm, w_ple, w_ple_gate, final_norm):
    lb_all = jnp.cumsum(jax.nn.softmax(hgrn_lb_logits.astype(F32), axis=0), axis=0)
    lb_all = lb_all - lb_all[0]
    split_idx = np.cumsum(IN_WIDTHS)[:-1].tolist()
    for i in range(DEPTH):
        h = rmsnorm(x, mix_norm[i])
        (a_z, a_xbc, a_dt, b_q, b_f, b_i, b_g, c_q, c_k, c_v, c_o, c_i, c_f,
         d_x, d_g, gate_pre) = jnp.split(h @ w_in[i], split_idx, axis=-1)
        y_a = mamba2_mixer(a_z, a_xbc, a_dt, ssm_conv_w[i], ssm_conv_b[i], ssm_dt_bias[i], ssm_a_log[i], ssm_d[i], ssm_norm[i])
        y_b = hgrn2_mixer(b_q, b_f, b_i, b_g, lb_all[i], hgrn_norm[i])
        y_c = mlstm_mixer(c_q, c_k, c_v, c_o, c_i, c_f, mlstm_i_bias[i], mlstm_f_bias[i], mlstm_norm[i])
        y_d = rglru_mixer(d_x, d_g, lru_conv_w[i], lru_conv_b[i], lru_wa[i], lru_ba[i], lru_wi[i], lru_bi[i], lru_a_param[i])
        ys = (y_a, y_b, y_c, y_d)
        merged = jnp.zeros_like(x)
        for br in range(N_BRANCH):
            gate = jax.nn.sigmoid(gate_pre[..., br * D_MODEL:(br + 1) * D_MODEL])
            merged = merged + gate * (ys[br].astype(x.dtype) @ w_branch[i, br])
        x = x + merged @ w_out[i]
        h = rmsnorm(x, mlp_norm[i])
        x = x + jnp.square(jax.nn.relu(h @ w_up[i])) @ w_down[i]
        h = rmsnorm(x, ple_norm[i])
        x = x + (p[i] @ w_ple[i]) * jax.nn.sigmoid(h @ w_ple_gate[i])
    return rmsnorm(x, final_norm)
```

```python
import math
import numpy as np
import concourse.bass as bass
import concourse.mybir as mybir
from concourse.bass_utils import run_bass_kernel_spmd

F32 = mybir.dt.float32
BF16 = mybir.dt.bfloat16
AF = mybir.ActivationFunctionType
ALU = mybir.AluOpType
AX = mybir.AxisListType

D = 2048
SEQ = 4096
DIN = 20504
DFF = 8192
TT = 256
REV = False
XTE = 'act'
NB = TT // 128
EPS = 1e-6

WIN_GROUPS = [
    ("a_z0", 0, 512), ("a_z1", 512, 512), ("a_dt", 3072, 16),
    ("a_x0", 1024, 512), ("a_x1", 1536, 512), ("a_B", 2048, 512), ("a_C", 2560, 512),
    ("b_q0", 3088, 512), ("b_q1", 3600, 512), ("b_f0", 4112, 512), ("b_f1", 4624, 512),
    ("b_i0", 5136, 512), ("b_i1", 5648, 512), ("b_g0", 6160, 512), ("b_g1", 6672, 512),
    ("c_if", 10256, 8), ("c_q", 7184, 512), ("c_k", 7696, 512), ("c_v0", 8208, 512), ("c_v1", 8720, 512),
    ("c_o0", 9232, 512), ("c_o1", 9744, 512),
    ("d_x0", 10264, 512), ("d_x1", 10776, 512), ("d_g0", 11288, 512), ("d_g1", 11800, 512),
] + [("gate%d_%d" % (br, j), 12312 + br * 2048 + j * 512, 512) for br in range(4) for j in range(4)]
WIN_OFF = {}
_o = 0
for _n, _c0, _w in WIN_GROUPS:
    WIN_OFF[_n] = (_o, _w)
    _o += 16 * _w
WIN_TOT = _o

C_MIXN, C_MLPN, C_PLEN, C_SCW, C_SCB, C_SNRM, C_HNRM, C_MNRM = 0, 16, 32, 48, 112, 128, 136, 144
C_LCW, C_LCB, C_LBA, C_LBI, C_LAP, C_LB0, C_LB1, NCOL = 152, 184, 192, 200, 208, 216, 224, 232
R_DTB, R_ALOG, R_DSK, R_IFB, NROW = 0, 16, 32, 48, 56


class _Stop(Exception):
    pass


class Buf:
    __slots__ = ("name", "w", "r", "psum")

    def __init__(self, name):
        self.name = name
        self.w = None
        self.r = {}
        self.psum = False


class V:
    __slots__ = ("ap", "buf")

    def __init__(self, ap, buf):
        self.ap = ap
        self.buf = buf

    def __getitem__(self, idx):
        return V(self.ap[idx], self.buf)

    def m(self, fn):
        return V(fn(self.ap), self.buf)

    def bc(self, shape):
        return V(self.ap.to_broadcast(list(shape)), self.buf)

    def ubc(self, axis, shape):
        return V(self.ap.unsqueeze(axis).to_broadcast(list(shape)), self.buf)

    def re(self, s, **kw):
        return V(self.ap.rearrange(s, **kw), self.buf)

    def cast(self, dt):
        return V(self.ap.bitcast(dt), self.buf)


def _merge(d, tok):
    sid = id(tok[0])
    o = d.get(sid)
    if o is None or o[1] < tok[1]:
        d[sid] = tok


class K:
    NDMA = 6

    def __init__(self, nc):
        self.nc = nc
        self.eng = {'pe': nc.tensor, 'act': nc.scalar, 'dve': nc.vector, 'pool': nc.gpsimd, 'sp': nc.sync}
        self.sem = {e: nc.alloc_semaphore(name="sem_" + e) for e in self.eng}
        self.cnt = {e: 0 for e in self.eng}
        self.waited = {e: {} for e in self.eng}
        self.dsem = {}
        self.dcnt = {}
        self.dnext = {}
        for q in ('sp', 'pool'):
            self.dsem[q] = [nc.alloc_semaphore(name="dma_%s%d" % (q, i)) for i in range(self.NDMA)]
            self.dcnt[q] = [0] * self.NDMA
            self.dnext[q] = 0
        self.ninstr = 0
        self.limit = None
        self.streams = {e: [] for e in self.eng}

    def sb(self, name, shape, dt):
        t = self.nc.alloc_sbuf_tensor(name, list(shape), dt)
        return V(t[tuple(slice(None) for _ in shape)], Buf(name))

    def ps(self, name, shape, dt):
        t = self.nc.alloc_psum_tensor(name, list(shape), dt)
        b = Buf(name)
        b.psum = True
        return V(t[tuple(slice(None) for _ in shape)], b)

    def dram(self, name, shape, dt, kind="Internal"):
        t = self.nc.dram_tensor(name, list(shape), dt, kind=kind)
        return V(t.ap(), Buf(name))

    def _wait(self, e, tok):
        sem, val = tok
        key = id(sem)
        if self.waited[e].get(key, 0) >= val:
            return
        self.eng[e].wait_ge(sem, val)
        self.streams[e].append(('w', key, val))
        self.waited[e][key] = val
        self.ninstr += 1

    def _deps(self, e, reads, writes, skip_own=False):
        own = id(self.sem[e])
        for b in reads:
            if b.w is not None and not (skip_own and id(b.w[0]) == own):
                self._wait(e, b.w)
            if b.psum:
                for sid, tok in b.r.items():
                    if sid != own:
                        self._wait(e, tok)
        for b in writes:
            if b.w is not None and not (skip_own and id(b.w[0]) == own):
                self._wait(e, b.w)
            for sid, tok in b.r.items():
                if skip_own and sid == own:
                    continue
                self._wait(e, tok)

    def _commit(self, tok, reads, writes):
        for b in reads:
            _merge(b.r, tok)
        for b in writes:
            b.w = tok
            b.r = {}

    def op(self, e, name, inc=True, **kw):
        reads = []
        writes = []
        args = {}
        for key, v in kw.items():
            if isinstance(v, V):
                (writes if key in ('out', 'accum_out', 'ap') else reads).append(v.buf)
                args[key] = v.ap
            else:
                args[key] = v
        if self.limit is not None and self.ninstr >= self.limit:
            raise _Stop()
        self._deps(e, reads, writes, skip_own=(e == 'pe'))
        ins = getattr(self.eng[e], name)(**args)
        self.ninstr += 1
        if inc:
            self.cnt[e] += 1
            ins.then_inc(self.sem[e], 1)
            self.streams[e].append(('i', id(self.sem[e]), 1))
            tok = (self.sem[e], self.cnt[e])
        else:
            tok = (self.sem[e], self.cnt[e] + 1)
        self._commit(tok, reads, writes)
        return ins

    def dma(self, q, out, in_, **kw):
        s = self.dnext[q]
        self.dnext[q] = (s + 1) % self.NDMA
        sem = self.dsem[q][s]
        if self.dcnt[q][s] > 0:
            self._wait(q, (sem, 16 * self.dcnt[q][s]))
        self._deps(q, [in_.buf], [out.buf])
        ins = self.eng[q].dma_start(out=out.ap, in_=in_.ap, **kw)
        self.ninstr += 1
        self.dcnt[q][s] += 1
        ins.then_inc(sem, 16)
        self.streams[q].append(('i', id(sem), 16))
        tok = (sem, 16 * self.dcnt[q][s])
        self._commit(tok, [in_.buf], [out.buf])
        return tok

    def finish(self):
        for q in self.dsem:
            for i, sem in enumerate(self.dsem[q]):
                if self.dcnt[q][i] > 0:
                    self._wait('sp', (sem, 16 * self.dcnt[q][i]))


def check_deadlock(k):
    pos = {e: 0 for e in k.streams}
    val = {}
    progress = True
    while progress:
        progress = False
        for e, st in k.streams.items():
            while pos[e] < len(st):
                kind, sid, v = st[pos[e]]
                if kind == 'w':
                    if val.get(sid, 0) >= v:
                        pos[e] += 1
                        progress = True
                    else:
                        break
                else:
                    val[sid] = val.get(sid, 0) + v
                    pos[e] += 1
                    progress = True
    stuck = {e: (pos[e], len(st)) for e, st in k.streams.items() if pos[e] < len(st)}
    return stuck


class Arena:
    def __init__(self, k, name, nwords):
        self.k = k
        self.base = k.sb(name, [128, nwords], F32)
        self.nwords = nwords
        self.live = [self.base.buf]

    def carve(self, specs):
        toks = {}
        for b in self.live:
            if b.w is not None:
                _merge(toks, b.w)
            for t in b.r.values():
                _merge(toks, t)
        self.live = []
        out = {}
        off = 0
        for name, dt, n in specs:
            words = n if dt == F32 else (n + 1) // 2
            assert off + words <= self.nwords, (name, off, words, self.nwords)
            ap = self.base.ap[:, off:off + words]
            if dt != F32:
                ap = ap.bitcast(dt)[:, 0:n]
            b = Buf(name)
            b.r = dict(toks)
            self.live.append(b)
            out[name] = V(ap, b)
            off += words
        return out


class WStream:
    def __init__(self, k, nslots, slot_elems):
        self.k = k
        self.slots = [k.sb("wt%d" % i, [128, slot_elems], BF16) for i in range(nslots)]
        self.n = nslots
        self.plan = []
        self.tok = {}
        self.issued = 0
        self.cur = 0

    def _issue(self, i):
        name, src, scr, off, ln, first, key = self.plan[i]
        slot = self.slots[i % self.n][:, 0:ln]
        if first:
            self.k.dma('pool', slot, src[:, off:off + ln])
            self.tok[key] = self.k.dma('sp', V(scr.ap[:, off:off + ln], Buf("wscr_w")), slot)
        else:
            self.k._wait('sp', self.tok[key])
            self.k.dma('sp', slot, V(scr.ap[:, off:off + ln], Buf("wscr")))

    def next(self, name, keep_prev=False):
        i = self.cur
        lim = min(len(self.plan), i + self.n - (1 if keep_prev else 0))
        while self.issued < lim:
            self._issue(self.issued)
            self.issued += 1
        assert self.plan[i][0] == name, (self.plan[i][0], name)
        self.cur += 1
        return self.slots[i % self.n]


def layer_plan(wd, l):
    P = []

    def win(nm):
        o, w = WIN_OFF[nm]
        P.append((nm, wd['win'][l], o, 16 * w))

    def merge(br):
        for j in range(4):
            win("gate%d_%d" % (br, j))
            P.append(("wbr%d_%d" % (br, j), wd['wbr'][l], (br * 4 + j) * 8 * 512, 8 * 512))

    for nm in ("a_dt", "a_x0", "a_x1", "a_z0", "a_B", "a_C", "a_z1"):
        win(nm)
    for nm in ("b_q0", "b_q1", "b_f0", "b_f1", "b_i0", "b_i1", "b_g0", "b_g1"):
        win(nm)
    merge(0)
    for nm in ("c_if", "c_q", "c_k", "c_v0", "c_v1", "c_o0", "c_o1"):
        win(nm)
    merge(1)
    for nm in ("d_x0", "d_x1"):
        win(nm)
    merge(2)
    for nm in ("d_g0", "d_g1"):
        win(nm)
    merge(3)
    for j in range(4):
        P.append(("wout_%d" % j, wd['wout'][l], j * 16 * 512, 16 * 512))
    for q in range(4):
        for j in range(4):
            P.append(("wup_%d" % (q * 4 + j), wd['wup'][l], (q * 4 + j) * 16 * 512, 16 * 512))
        for j in range(4):
            P.append(("wdn_%d_%d" % (q, j), wd['wdn'][l], (q * 4 + j) * 16 * 512, 16 * 512))
    for j in range(4):
        P.append(("wpg_%d" % j, wd['wpg'][l], j * 16 * 512, 16 * 512))
        P.append(("wple_%d" % j, wd['wple'][l], j * 2 * 512, 2 * 512))
    return P


def build(ntiles=SEQ // TT, nlayers=2, taps=False, stop=None, limit=None):
    nc = bass.Bass("TRN2", target_bir_lowering=False)
    k = K(nc)
    k.limit = limit
    op = k.op
    ntok = ntiles * TT

    x_d = k.dram("x", [SEQ, D], F32, "ExternalInput")
    pT_d = k.dram("pT", [2, 256, SEQ], F32, "ExternalInput")
    wd = {
        'win': k.dram("win", [2, 128, WIN_TOT], F32, "ExternalInput"),
        'wbr': k.dram("wbr", [2, 128, 4 * 8 * 2048], F32, "ExternalInput"),
        'wout': k.dram("wout", [2, 128, 16 * 2048], F32, "ExternalInput"),
        'wup': k.dram("wup", [2, 128, 16 * DFF], F32, "ExternalInput"),
        'wdn': k.dram("wdn", [2, 128, 64 * 2048], F32, "ExternalInput"),
        'wple': k.dram("wple", [2, 128, 2 * 2048], F32, "ExternalInput"),
        'wpg': k.dram("wpg", [2, 128, 16 * 2048], F32, "ExternalInput"),
    }
    lruw_d = k.dram("lruw", [2, 128, 2 * 8 * 128], F32, "ExternalInput")
    colp_d = k.dram("colp", [2, 128, NCOL], F32, "ExternalInput")
    rowp_d = k.dram("rowp", [2, 1, NROW], F32, "ExternalInput")
    fnorm_d = k.dram("fnorm", [1, D], F32, "ExternalInput")
    out_d = k.dram("out", [SEQ, D], F32, "ExternalOutput")
    tap_outs = []

    def tap(name, v, shape, dt=F32):
        if not taps:
            return
        t = k.dram("tap_" + name, list(shape), dt, "ExternalOutput")
        k.dma('sp', t, v)
        tap_outs.append(t)

    xs = [k.sb("xs%d" % b, [128, D], F32) for b in range(NB)]
    hT = k.sb("hT", [128, 16 * TT], BF16)
    hT3 = hT.re("p (c t) -> p c t", t=TT)
    macc = k.sb("macc", [128, 16 * TT], F32)
    macc3 = macc.re("p (c t) -> p c t", t=TT)
    aq = macc.cast(BF16)[:, 0:16 * TT]
    aq3 = aq.re("p (c t) -> p c t", t=TT)
    ybrs = [k.sb("ybr%d" % i, [128, 8 * TT], BF16) for i in range(2)]
    ybr3s = [y.re("p (c t) -> p c t", t=TT) for y in ybrs]
    cur = {'br': 0}
    pending = {'gen': None}

    def filler(n=1):
        for _ in range(n):
            g = pending['gen']
            if g is None:
                return
            try:
                next(g)
            except StopIteration:
                pending['gen'] = None

    def drain():
        while pending['gen'] is not None:
            filler()
    ws = WStream(k, 3, 16 * 512)
    colp = [k.sb("colp%d" % l, [128, NCOL], F32) for l in range(2)]
    rowp = [k.sb("rowp%d" % l, [128, NROW], F32) for l in range(2)]
    arow = [k.sb("arow%d" % l, [128, 16], F32) for l in range(2)]
    lbp = [k.sb("lbp%d" % l, [128, 16], F32) for l in range(2)]
    lrup = [k.sb("lrup%d" % l, [128, 16], F32) for l in range(2)]
    lruw = [k.sb("lruw%d" % l, [128, 2 * 8 * 128], BF16) for l in range(2)]
    ident = k.sb("ident", [128, 128], BF16)
    identf = k.sb("identf", [128, 128], F32)
    Umat = k.sb("Umat", [128, 128], F32)
    negU = k.sb("negU", [128, 128], F32)
    NEGM = k.sb("NEGM", [128, 128], F32)
    ones = k.sb("ones", [128, TT], F32)
    hmask = k.sb("hmask", [128, 128], F32)
    evn = k.sb("evn", [128, TT], BF16)
    odd = k.sb("odd", [128, TT], BF16)
    sm = k.sb("sm", [128, 512], F32)
    sm2 = k.sb("sm2", [128, 256], F32)
    sm_ecs = k.sb("sm_ecs", [128, 16], F32)
    sm_dst = k.sb("sm_dst", [128, 16], F32)
    sm_etot = k.sb("sm_etot", [128, 16], F32)
    ss = k.sb("ss", [128, 8], F32)
    hgp = k.sb("hgp", [128, 128], F32)
    st_ssd = [k.sb("st_ssd%d" % l, [128, 1024], F32) for l in range(2)]
    st_ml = [k.sb("st_ml%d" % l, [128, 4 * 257], F32) for l in range(2)]
    st_hg = [k.sb("st_hg%d" % l, [128, 1024], F32) for l in range(2)]
    st_lru = [k.sb("st_lru%d" % l, [128, 8], F32) for l in range(2)]
    tail_ssd = [k.sb("tail_ssd%d" % l, [128, 16 * 3], F32) for l in range(2)]
    tail_lru = [k.sb("tail_lru%d" % l, [128, 8 * 3], F32) for l in range(2)]
    stb = k.sb("stb", [128, 4 * 257 + 4], BF16)
    sb0 = k.sb("sb0", [128, 1024], BF16)
    sb1 = k.sb("sb1", [128, 1024], BF16)
    CINW = 8 * (TT + 3)
    arena = Arena(k, "arena", 14400)
    banks = [k.ps("bank%d" % i, [128, 512], F32) for i in range(8)]
    bstate = {'i': 0}

    def pb():
        b = banks[bstate['i'] % 8]
        bstate['i'] += 1
        return b

    op('dve', 'memset', ap=identf, constant=1.0)
    op('pool', 'affine_select', out=identf, in_=identf, pattern=[[-1, 128]], compare_op=ALU.is_equal, fill=0.0,
       base=0, channel_multiplier=1)
    op('dve', 'tensor_copy', out=ident, in_=identf)
    op('dve', 'memset', ap=Umat, constant=1.0)
    op('pool', 'affine_select', out=Umat, in_=Umat, pattern=[[1, 128]], compare_op=ALU.is_ge, fill=0.0,
       base=0, channel_multiplier=-1)
    op('dve', 'tensor_scalar', out=negU, in0=Umat, scalar1=-1.0, scalar2=None, op0=ALU.mult)
    op('dve', 'memset', ap=NEGM, constant=-30000.0)
    op('pool', 'affine_select', out=NEGM, in_=NEGM, pattern=[[-1, 128]], compare_op=ALU.is_gt, fill=0.0,
       base=0, channel_multiplier=1)
    op('dve', 'memset', ap=ones, constant=1.0)
    op('dve', 'tensor_copy', out=hmask, in_=Umat)
    op('dve', 'memset', ap=hmask[0:64, 64:128], constant=0.0)
    op('dve', 'memset', ap=evn, constant=1.0)
    op('dve', 'memset', ap=evn.re("p (a two c) -> p a two c", two=2, c=64)[:, :, 1, :], constant=0.0)
    op('dve', 'memset', ap=odd, constant=0.0)
    op('dve', 'memset', ap=odd.re("p (a two c) -> p a two c", two=2, c=64)[:, :, 1, :], constant=1.0)
    for l in range(2):
        for t in (st_ssd[l], st_ml[l], st_hg[l], st_lru[l], tail_ssd[l], tail_lru[l]):
            op('dve', 'memset', ap=t, constant=0.0)
        k.dma('sp', colp[l], colp_d[l])
        k.dma('sp', rowp[l], rowp_d[l].m(lambda a: a.partition_broadcast(128)))
        k.dma('pool', lruw[l], lruw_d[l])
        op('act', 'activation', out=arow[l], in_=rowp[l][:, R_ALOG:R_ALOG + 16], func=AF.Exp)
        op('dve', 'tensor_scalar', out=arow[l], in0=arow[l], scalar1=-1.0, scalar2=None, op0=ALU.mult)
        op('act', 'activation', out=lrup[l][:, 0:8], in_=colp[l][:, C_LAP:C_LAP + 8], func=AF.Exp, scale=-1.0)
        op('act', 'activation', out=lrup[l][:, 0:8], in_=lrup[l][:, 0:8], func=AF.Ln, bias=1.0)
        op('dve', 'tensor_scalar', out=lrup[l][:, 8:16], in0=lrup[l][:, 0:8], scalar1=-16.0, scalar2=None, op0=ALU.mult)
        op('dve', 'tensor_scalar', out=lrup[l][:, 0:8], in0=lrup[l][:, 0:8], scalar1=-8.0, scalar2=None, op0=ALU.mult)
    op('dve', 'memset', ap=lbp[0][:, 0:8], constant=0.0)
    op('dve', 'memset', ap=lbp[0][:, 8:16], constant=1.0)
    op('dve', 'tensor_tensor', out=lbp[1][:, 0:8], in0=colp[0][:, C_LB0:C_LB0 + 8], in1=colp[0][:, C_LB1:C_LB1 + 8],
       op=ALU.subtract)
    op('act', 'activation', out=lbp[1][:, 0:8], in_=lbp[1][:, 0:8], func=AF.Sigmoid)
    op('dve', 'tensor_scalar', out=lbp[1][:, 8:16], in0=lbp[1][:, 0:8], scalar1=-1.0, scalar2=1.0, op0=ALU.mult,
       op1=ALU.add)

    wscr = {nm: k.dram(nm + "_bf16", [2, 128, v.ap.shape[2]], BF16) for nm, v in wd.items()}
    first_plan = {}
    for l in range(nlayers):
        ents = []
        for name, src, off, ln in layer_plan(wd, l):
            key = [nm for nm, v in wd.items() if v.buf is src.buf][0]
            ents.append((name, key, off, ln))
        first_plan[l] = ents
    plan = []
    for t in range(ntiles):
        for l in range(nlayers):
            for name, key, off, ln in first_plan[l]:
                plan.append((name, wd[key][l], wscr[key][l], off, ln, t == 0, (l, name)))
    ws.plan = plan

    def ck(name):
        if stop == name:
            raise _Stop()

    def w3(wt, kc, n):
        return wt[:, 0:kc * n].re("p (k c) -> p k c", c=n)

    def proj_fm(wt, n, evac, rhs3=None, kc=16):
        w = w3(wt, kc, n)
        rhs3 = hT3 if rhs3 is None else rhs3
        for j in range(n // 128):
            ps = pb()
            for c in range(kc):
                op('pe', 'matmul', inc=(c == kc - 1), out=ps[:, 0:TT], lhsT=w[:, c, j * 128:(j + 1) * 128],
                   rhs=rhs3[:, c, :], start=(c == 0), stop=(c == kc - 1))
            evac(j, ps[:, 0:TT])

    def proj_tm(wt, n, evac, lhs3=None, kc=16):
        w = w3(wt, kc, n)
        lhs3 = hT3 if lhs3 is None else lhs3
        for b in range(NB):
            ps = pb()
            for c in range(kc):
                op('pe', 'matmul', inc=(c == kc - 1), out=ps[:, 0:n], lhsT=lhs3[:, c, b * 128:(b + 1) * 128],
                   rhs=w[:, c, :], start=(c == 0), stop=(c == kc - 1))
            evac(b, ps[:, 0:n])

    def rmsnorm_hT(l, coff):
        A = arena.carve([("xn%d" % b, BF16, D) for b in range(NB)])
        xn = [A["xn%d" % b] for b in range(NB)]
        for b in range(NB):
            op('act', 'activation', out=xn[b], in_=xs[b], func=AF.Square, accum_out=ss[:, b:b + 1])
            op('act', 'activation', out=ss[:, 4 + b:5 + b], in_=ss[:, b:b + 1], func=AF.Sqrt, scale=1.0 / D, bias=EPS)
            op('dve', 'reciprocal', out=ss[:, 4 + b:5 + b], in_=ss[:, 4 + b:5 + b])
            op('dve', 'tensor_scalar', out=xn[b], in0=xs[b], scalar1=ss[:, 4 + b:5 + b], scalar2=None, op0=ALU.mult)
            for half in range(2):
                pt = pb().cast(BF16)
                for c in range(8):
                    cc = half * 8 + c
                    op('pe', 'transpose', inc=(c == 7), out=pt[:, c * 128:(c + 1) * 128],
                       in_=xn[b][:, cc * 128:(cc + 1) * 128], identity=ident)
                op('dve', 'tensor_tensor', out=hT3[:, half * 8:half * 8 + 8, b * 128:(b + 1) * 128],
                   in0=pt.re("p (c t) -> p c t", t=128),
                   in1=colp[l][:, coff + half * 8:coff + half * 8 + 8].ubc(2, [128, 8, 128]), op=ALU.mult)

    def conv_chunk(src, wcol, bcol, acc):
        op('dve', 'tensor_scalar', out=acc, in0=src[:, 0:TT], scalar1=wcol(0), scalar2=bcol, op0=ALU.mult,
           op1=ALU.add)
        for t in range(1, 4):
            op('dve', 'scalar_tensor_tensor', out=acc, in0=src[:, t:t + TT], scalar=wcol(t), in1=acc,
               op0=ALU.mult, op1=ALU.add)

    def lin_attn_chunk(H, G, P, adt, qF, kF, kT, vT2, st2, stb2, y2, A):
        R = H // G
        v3 = lambda v: v.re("p (h q) -> p h q", q=P)
        pc = pb()
        op('pe', 'matmul', inc=False, out=pc[:, 0:H], lhsT=Umat, rhs=adt, start=True, stop=True)
        op('pe', 'matmul', out=pc[:, H:2 * H], lhsT=ones[:, 0:128], rhs=adt, start=True, stop=True)
        csb = sm[:, 0:2 * H]
        op('dve', 'tensor_copy', out=csb, in_=pc[:, 0:2 * H])
        cs, tot = sm[:, 0:H], sm[:, H:2 * H]
        ecs, dst, etot = sm_ecs[:, 0:H], sm_dst[:, 0:H], sm_etot[:, 0:H]
        op('act', 'activation', out=ecs, in_=cs, func=AF.Exp)
        op('dve', 'tensor_tensor', out=dst, in0=tot, in1=cs, op=ALU.subtract)
        op('act', 'activation', out=dst, in_=dst, func=AF.Exp)
        op('act', 'activation', out=etot, in_=tot, func=AF.Exp)
        vw = A['vw'][:, 0:H * P]
        op('dve', 'tensor_tensor', out=v3(vw), in0=v3(vT2), in1=dst.ubc(2, [128, H, P]), op=ALU.mult)
        pg = pb()
        for g in range(G):
            op('pe', 'matmul', inc=(g == G - 1), out=pg[:, g * 128:(g + 1) * 128], lhsT=kF(g), rhs=qF(g), start=True,
               stop=True)
        MT = A['MT']
        MT3 = MT.re("p (h t) -> p h t", t=128)
        for hb in range(H // 4):
            pd = pb()
            for i in range(4):
                h = hb * 4 + i
                o = pd[:, i * 128:(i + 1) * 128]
                op('pe', 'matmul', inc=False, out=o, lhsT=adt[:, h:h + 1].bc([128, 128]), rhs=Umat, start=True,
                   stop=False)
                op('pe', 'matmul', inc=False, out=o, lhsT=negU, rhs=adt[:, h:h + 1].bc([128, 128]), start=False,
                   stop=False)
                op('pe', 'matmul', inc=(i == 3), out=o, lhsT=identf, rhs=NEGM, start=False, stop=True)
            E = A['E%d' % (hb % 2)]
            op('act', 'activation', out=E, in_=pd, func=AF.Exp)
            if R == 4:
                gin = pg[:, hb * 128:(hb + 1) * 128].ubc(1, [128, 4, 128])
            else:
                gin = pg.re("p (h t) -> p h t", t=128)
            op('dve', 'tensor_tensor', out=MT3[:, hb * 4:hb * 4 + 4, :], in0=E.re("p (h t) -> p h t", t=128), in1=gin,
               op=ALU.mult)
            filler(1)
        HPB = max(1, 512 // P)
        for bk in range((H + HPB - 1) // HPB):
            hs = list(range(bk * HPB, min(H, (bk + 1) * HPB)))
            n = len(hs)
            pyd = pb()
            pyo = pb()
            for idx, h in enumerate(hs):
                op('pe', 'matmul', inc=(idx == n - 1), out=pyd[:, idx * P:(idx + 1) * P], lhsT=MT3[:, h, :],
                   rhs=vT2[:, h * P:(h + 1) * P], start=True, stop=True)
            for idx, h in enumerate(hs):
                op('pe', 'matmul', inc=(idx == n - 1), out=pyo[:, idx * P:(idx + 1) * P], lhsT=qF(h // R),
                   rhs=stb2[:, h * P:(h + 1) * P], start=True, stop=True)
            yv = v3(y2)[:, hs[0]:hs[0] + n, :]
            op('dve', 'tensor_tensor', out=yv, in0=pyo[:, 0:n * P].re("p (h q) -> p h q", q=P),
               in1=ecs[:, hs[0]:hs[0] + n].ubc(2, [128, n, P]), op=ALU.mult)
            op('dve', 'tensor_tensor', out=yv, in0=yv, in1=pyd[:, 0:n * P].re("p (h q) -> p h q", q=P), op=ALU.add)
            filler(1)
        GPB = max(1, 512 // (R * P))
        for bk in range((G + GPB - 1) // GPB):
            gs = list(range(bk * GPB, min(G, (bk + 1) * GPB)))
            pu = pb()
            for idx, g in enumerate(gs):
                op('pe', 'matmul', inc=(idx == len(gs) - 1), out=pu[:, idx * R * P:(idx + 1) * R * P], lhsT=kT(g),
                   rhs=vw[:, g * R * P:(g + 1) * R * P], start=True, stop=True)
            h0, nh = gs[0] * R, len(gs) * R
            sv = v3(st2)[:, h0:h0 + nh, :]
            op('dve', 'tensor_tensor', out=sv, in0=sv, in1=etot[:, h0:h0 + nh].ubc(2, [128, nh, P]), op=ALU.mult)
            op('dve', 'tensor_tensor', out=sv, in0=sv, in1=pu[:, 0:nh * P].re("p (h q) -> p h q", q=P), op=ALU.add)
            op('act', 'activation', out=v3(stb2)[:, h0:h0 + nh, :], in_=sv, func=AF.Copy)
            filler(1)

    def post_norm_T(l, b, y, ng, gate_after, coff, A):
        gsz = 1024 // ng
        sq = A['sq']
        y3 = y.re("p (g c) -> p g c", g=ng)
        op('dve', 'tensor_tensor', out=sq, in0=y, in1=y, op=ALU.mult)
        sg = sm2[:, 0:ng]
        op('dve', 'tensor_reduce', out=sg, in_=sq.re("p (g c) -> p g c", g=ng), axis=AX.X, op=ALU.add)
        op('act', 'activation', out=sg, in_=sg, func=AF.Sqrt, scale=1.0 / gsz, bias=EPS)
        op('dve', 'reciprocal', out=sg, in_=sg)
        yn = A['yn']
        if gate_after is None:
            op('dve', 'tensor_tensor', out=yn.re("p (g c) -> p g c", g=ng), in0=y3, in1=sg.ubc(2, [128, ng, gsz]),
               op=ALU.mult)
        else:
            op('dve', 'tensor_tensor', out=y3, in0=y3, in1=sg.ubc(2, [128, ng, gsz]), op=ALU.mult)
            op('dve', 'tensor_tensor', out=yn, in0=y, in1=gate_after, op=ALU.mult)
        br_now = cur['br']

        def part2():
            pt = pb().cast(BF16)
            for c in range(8):
                op('pe', 'transpose', inc=(c == 7), out=pt[:, c * 128:(c + 1) * 128], in_=yn[:, c * 128:(c + 1) * 128],
                   identity=ident)
            op('dve', 'tensor_tensor', out=ybr3s[br_now % 2][:, :, b * 128:(b + 1) * 128],
               in0=pt.re("p (c t) -> p c t", t=128), in1=colp[l][:, coff:coff + 8].ubc(2, [128, 8, 128]), op=ALU.mult)
        return part2

    def merge_gen(l, br):
        yb3 = ybr3s[br % 2]
        for j in range(4):
            wg = w3(ws.next("gate%d_%d" % (br, j)), 16, 512)
            wb = w3(ws.next("wbr%d_%d" % (br, j), keep_prev=True), 8, 512)
            for c in range(4):
                jc = j * 4 + c
                pG = pb()
                for kc in range(16):
                    op('pe', 'matmul', inc=(kc == 15), out=pG[:, 0:TT], lhsT=wg[:, kc, c * 128:(c + 1) * 128],
                       rhs=hT3[:, kc, :], start=(kc == 0), stop=(kc == 15))
                pZ = pb()
                for kc in range(8):
                    op('pe', 'matmul', inc=(kc == 7), out=pZ[:, 0:TT], lhsT=wb[:, kc, c * 128:(c + 1) * 128],
                       rhs=yb3[:, kc, :], start=(kc == 0), stop=(kc == 7))
                sg = sgb[jc % 2][:, 0:TT]
                op('act', 'activation', out=sg, in_=pG[:, 0:TT], func=AF.Sigmoid)
                if br == 0:
                    op('dve', 'tensor_tensor', out=macc3[:, jc, :], in0=sg, in1=pZ[:, 0:TT], op=ALU.mult)
                else:
                    op('dve', 'tensor_tensor', out=sg, in0=sg, in1=pZ[:, 0:TT], op=ALU.mult)
                    op('pool', 'tensor_tensor', out=macc3[:, jc, :], in0=macc3[:, jc, :], in1=sg, op=ALU.add)
                yield

    def merge(l, br, defer=True):
        drain()
        pending['gen'] = merge_gen(l, br)
        if not defer:
            drain()

    sgb = [k.sb("sgb%d" % i, [128, 512], F32) for i in range(2)]

    def mixer_ssd(l):
        A = arena.carve([("zs", BF16, NB * 1024), ("xF", BF16, 8 * TT), ("BF", BF16, 4 * TT), ("CF", BF16, 4 * TT),
                         ("xT", BF16, NB * 1024), ("xdt", BF16, NB * 1024), ("BT", BF16, NB * 512),
                         ("dt", F32, NB * 16), ("adt", F32, NB * 16),
                         ("vw", BF16, 1024), ("E0", F32, 512), ("E1", F32, 512), ("MT", BF16, 16 * 128),
                         ("y", F32, 1024), ("sq", F32, 1024), ("yn", BF16, 1024), ("acc0", F32, TT),
                         ("acc1", F32, TT)] + [("cin%d" % j, F32, TT + 3) for j in range(8)])
        cinj = [A["cin%d" % j] for j in range(8)]
        accs = [A['acc0'], A['acc1']]
        zs, xF, BFm, CFm = A['zs'], A['xF'], A['BF'], A['CF']
        xF3 = xF.re("p (c t) -> p c t", t=TT)
        BF3 = BFm.re("p (c t) -> p c t", t=TT)
        CF3 = CFm.re("p (c t) -> p c t", t=TT)
        def zproj(gi):
            wt = ws.next("a_z%d" % gi)
            proj_tm(wt, 512, lambda b, ps, gi=gi: op('act', 'activation',
                                                     out=zs[:, b * 1024 + gi * 512:b * 1024 + gi * 512 + 512], in_=ps,
                                                     func=AF.Silu))

        wt = ws.next("a_dt")

        def ev_dt(b, ps):
            d = A['dt'][:, b * 16:(b + 1) * 16]
            op('dve', 'tensor_tensor', out=d, in0=ps, in1=rowp[l][:, R_DTB:R_DTB + 16], op=ALU.add)
            op('act', 'activation', out=d, in_=d, func=AF.Exp)
            op('act', 'activation', out=d, in_=d, func=AF.Ln, bias=1.0)
            op('dve', 'tensor_tensor', out=A['adt'][:, b * 16:(b + 1) * 16], in0=d, in1=arow[l], op=ALU.mult)
        ck('ssd1')
        proj_tm(wt, 16, ev_dt)
        ck('ssd2')
        for half, names in enumerate((("a_x0", "a_x1"), ("a_B", "a_C"))):
            tl3 = tail_ssd[l].re("p (c t) -> p c t", t=3)
            for gi, nm in enumerate(names):
                wt = ws.next(nm)

                def ev_x(j, ps, gi=gi, half=half):
                    jj = gi * 4 + j
                    ch = half * 8 + jj
                    op('dve', 'tensor_copy', out=cinj[jj][:, 0:3], in_=tl3[:, ch, :])
                    op('act', 'activation', out=cinj[jj][:, 3:3 + TT], in_=ps, func=AF.Copy)
                    op('dve', 'tensor_copy', out=tl3[:, ch, :], in_=cinj[jj][:, TT:TT + 3])
                proj_fm(wt, 512, ev_x)
            for j in range(8):
                ch = half * 8 + j
                acc = accs[j % 2]
                conv_chunk(cinj[j], lambda t, ch=ch: colp[l][:, C_SCW + ch * 4 + t:C_SCW + ch * 4 + t + 1],
                           colp[l][:, C_SCB + ch:C_SCB + ch + 1], acc)
                if half == 0:
                    dstv = xF3[:, j, :]
                else:
                    dstv = BF3[:, j, :] if j < 4 else CF3[:, j - 4, :]
                op('act', 'activation', out=dstv, in_=acc, func=AF.Silu)
            zproj(half)
        ck('ssd3')
        op('act', 'activation', out=stb[:, 0:1024], in_=st_ssd[l], func=AF.Copy)
        dfr = [None]
        for b in (range(NB) if not REV else reversed(range(NB))):
            blk = slice(b * 128, (b + 1) * 128)
            pt = pb().cast(BF16)
            for c in range(8):
                op('pe', 'transpose', inc=(c == 7), out=pt[:, c * 128:(c + 1) * 128], in_=xF3[:, c, blk], identity=ident)
            xTb = A['xT'][:, b * 1024:(b + 1) * 1024]
            op(XTE, 'tensor_copy', out=xTb, in_=pt) if XTE == 'dve' else op('act', 'activation', out=xTb, in_=pt, func=AF.Copy)
            xdtb = A['xdt'][:, b * 1024:(b + 1) * 1024]
            op('dve', 'tensor_tensor', out=xdtb.re("p (h q) -> p h q", q=64), in0=xTb.re("p (h q) -> p h q", q=64),
               in1=A['dt'][:, b * 16:(b + 1) * 16].ubc(2, [128, 16, 64]), op=ALU.mult)
            pt2 = pb().cast(BF16)
            for c in range(4):
                op('pe', 'transpose', inc=(c == 3), out=pt2[:, c * 128:(c + 1) * 128], in_=BF3[:, c, blk], identity=ident)
            BTb = A['BT'][:, b * 512:(b + 1) * 512]
            op('act', 'activation', out=BTb, in_=pt2[:, 0:512], func=AF.Copy)
            ck('ssd4')
            y = A['y']
            lin_attn_chunk(16, 4, 64, A['adt'][:, b * 16:(b + 1) * 16],
                           lambda g: CF3[:, g, blk], lambda g: BF3[:, g, blk],
                           lambda g: BTb[:, g * 128:(g + 1) * 128], xdtb, st_ssd[l], stb[:, 0:1024], y, A)
            ck('ssd5')
            sq = A['sq']
            op('dve', 'tensor_tensor', out=sq.re("p (h q) -> p h q", q=64), in0=xTb.re("p (h q) -> p h q", q=64),
               in1=rowp[l][:, R_DSK:R_DSK + 16].ubc(2, [128, 16, 64]), op=ALU.mult)
            op('dve', 'tensor_tensor', out=y, in0=y, in1=sq, op=ALU.add)
            op('dve', 'tensor_tensor', out=y, in0=y, in1=zs[:, b * 1024:(b + 1) * 1024], op=ALU.mult)
            ck('ssd6')
            if dfr[0] is not None:
                dfr[0]()
            dfr[0] = post_norm_T(l, b, y, 4, None, C_SNRM, A)
            ck('ssd7')
        dfr[0]()

    def mixer_mlstm(l):
        PP = 257
        A = arena.carve([("so", BF16, NB * 1024), ("qF", BF16, 4 * TT), ("kF", BF16, 4 * TT), ("kT", BF16, NB * 512),
                         ("v", BF16, NB * 4 * PP + 2), ("z8", F32, NB * 8), ("eig", F32, NB * 4), ("lf", F32, NB * 4),
                         ("vw", BF16, 4 * PP + 2), ("E0", F32, 512), ("E1", F32, 512), ("MT", BF16, 4 * 128),
                         ("y", F32, 4 * PP + 1), ("hh", F32, 1024), ("sq", F32, 1024), ("yn", BF16, 1024),
                         ("dd", F32, 8)])
        qF3 = A['qF'].re("p (c t) -> p c t", t=TT)
        kF3 = A['kF'].re("p (c t) -> p c t", t=TT)
        wt = ws.next("c_if")

        def ev_if(b, ps):
            z8 = A['z8'][:, b * 8:(b + 1) * 8]
            op('dve', 'tensor_tensor', out=z8, in0=ps, in1=rowp[l][:, R_IFB:R_IFB + 8], op=ALU.add)
            op('act', 'activation', out=A['eig'][:, b * 4:(b + 1) * 4], in_=z8[:, 0:4], func=AF.Exp)
            lf = A['lf'][:, b * 4:(b + 1) * 4]
            op('act', 'activation', out=lf, in_=z8[:, 4:8], func=AF.Exp, scale=-1.0)
            op('act', 'activation', out=lf, in_=lf, func=AF.Ln, bias=1.0)
            op('dve', 'tensor_scalar', out=lf, in0=lf, scalar1=-1.0, scalar2=None, op0=ALU.mult)
        proj_tm(wt, 8, ev_if)
        wt = ws.next("c_q")
        proj_fm(wt, 512, lambda j, ps: op('act', 'activation', out=qF3[:, j, :], in_=ps, func=AF.Copy,
                                          scale=128.0 ** -0.5))
        wt = ws.next("c_k")
        proj_fm(wt, 512, lambda j, ps: op('act', 'activation', out=kF3[:, j, :], in_=ps, func=AF.Copy))
        for gi in range(2):
            wt = ws.next("c_v%d" % gi)

            def ev_v(b, ps, gi=gi):
                vb = A['v'][:, b * 4 * PP:(b + 1) * 4 * PP].re("p (h q) -> p h q", q=PP)
                op('dve', 'tensor_tensor', out=vb[:, gi * 2:gi * 2 + 2, 0:256], in0=ps.re("p (h q) -> p h q", q=256),
                   in1=A['eig'][:, b * 4 + gi * 2:b * 4 + gi * 2 + 2].ubc(2, [128, 2, 256]), op=ALU.mult)
            proj_tm(wt, 512, ev_v)
        for b in range(NB):
            vb = A['v'][:, b * 4 * PP:(b + 1) * 4 * PP].re("p (h q) -> p h q", q=PP)
            op('dve', 'tensor_copy', out=vb[:, :, 256:257], in_=A['eig'][:, b * 4:(b + 1) * 4].ubc(2, [128, 4, 1]))
        for gi in range(2):
            wt = ws.next("c_o%d" % gi)
            proj_tm(wt, 512, lambda b, ps, gi=gi: op('act', 'activation',
                                                     out=A['so'][:, b * 1024 + gi * 512:b * 1024 + gi * 512 + 512],
                                                     in_=ps, func=AF.Sigmoid))
        op('act', 'activation', out=stb[:, 0:4 * PP], in_=st_ml[l], func=AF.Copy)
        dfr = [None]
        for b in range(NB):
            blk = slice(b * 128, (b + 1) * 128)
            pt = pb().cast(BF16)
            for c in range(4):
                op('pe', 'transpose', inc=(c == 3), out=pt[:, c * 128:(c + 1) * 128], in_=kF3[:, c, blk], identity=ident)
            kTb = A['kT'][:, b * 512:(b + 1) * 512]
            op('act', 'activation', out=kTb, in_=pt[:, 0:512], func=AF.Copy)
            y = A['y'][:, 0:4 * PP]
            lin_attn_chunk(4, 4, PP, A['lf'][:, b * 4:(b + 1) * 4],
                           lambda g: qF3[:, g, blk], lambda g: kF3[:, g, blk],
                           lambda g: kTb[:, g * 128:(g + 1) * 128], A['v'][:, b * 4 * PP:(b + 1) * 4 * PP],
                           st_ml[l], stb[:, 0:4 * PP], y, A)
            y3 = y.re("p (h q) -> p h q", q=PP)
            dd = A['dd'][:, 0:4]
            op('act', 'activation', out=dd.ubc(2, [128, 4, 1]) if False else dd, in_=y3[:, :, 256], func=AF.Abs)
            op('dve', 'tensor_scalar', out=dd, in0=dd, scalar1=1.0, scalar2=None, op0=ALU.max)
            op('dve', 'reciprocal', out=dd, in_=dd)
            hh = A['hh']
            op('dve', 'tensor_tensor', out=hh.re("p (h q) -> p h q", q=256), in0=y3[:, :, 0:256],
               in1=dd.ubc(2, [128, 4, 256]), op=ALU.mult)
            if dfr[0] is not None:
                dfr[0]()
            dfr[0] = post_norm_T(l, b, hh, 4, A['so'][:, b * 1024:(b + 1) * 1024], C_MNRM, A)
        dfr[0]()

    def mixer_hgrn(l):
        NCH = TT // 64
        A = arena.carve([("X1", F32, 8 * TT), ("X2", F32, 8 * TT), ("X3", F32, 8 * TT), ("X4", F32, 8 * TT),
                         ("X5", F32, 8 * TT), ("qt", BF16, 8 * TT), ("kt", BF16, 8 * TT),
                         ("vT", BF16, NB * 1024), ("gs", BF16, NB * 1024)])
        A['lam'], A['mu'], A['nu'], A['gst'] = (hgp[:, i * 32:i * 32 + 8 * NCH] for i in range(4))
        X1, X2, X3, X4, X5 = (A['X%d' % i].re("p (h t) -> p h t", t=TT) for i in range(1, 6))
        for gi in range(2):
            wt = ws.next("b_q%d" % gi)
            proj_fm(wt, 512, lambda j, ps, gi=gi: op('act', 'activation', out=X4[:, gi * 4 + j, :], in_=ps, func=AF.Silu))
        for gi in range(2):
            wt = ws.next("b_f%d" % gi)

            def ev_f(j, ps, gi=gi):
                h = gi * 4 + j
                op('act', 'activation', out=X1[:, h, :], in_=ps, func=AF.Sigmoid)
                op('dve', 'tensor_scalar', out=X1[:, h, :], in0=X1[:, h, :], scalar1=lbp[l][:, 8 + h:9 + h],
                   scalar2=lbp[l][:, h:h + 1], op0=ALU.mult, op1=ALU.add)
                op('dve', 'tensor_scalar', out=X2[:, h, :], in0=X1[:, h, :], scalar1=-1.0, scalar2=1.0, op0=ALU.mult,
                   op1=ALU.add)
                op('act', 'activation', out=X1[:, h, :], in_=X1[:, h, :], func=AF.Ln)
                op('dve', 'tensor_tensor_scan', out=X3[:, h, :], data0=ones, data1=X1[:, h, :], initial=0.0,
                   op0=ALU.mult, op1=ALU.add)
            proj_fm(wt, 512, ev_f)
        G4 = A['X3'].re("p (h c j) -> p h c j", c=NCH, j=64)
        Gc = A['X3'].re("p (hc j) -> p hc j", j=64)
        Gd = A['X5'].re("p (hc j) -> p hc j", j=64)
        op('dve', 'tensor_tensor', out=Gd, in0=Gc, in1=Gc[:, :, 31:32].bc([128, 8 * NCH, 64]), op=ALU.subtract)
        gst = A['gst'].re("p (h c) -> p h c", c=NCH)
        lam = A['lam'].re("p (h c) -> p h c", c=NCH)
        mu = A['mu'].re("p (h c) -> p h c", c=NCH)
        nu = A['nu'].re("p (h c) -> p h c", c=NCH)
        op('dve', 'memset', ap=A['gst'], constant=0.0)
        op('dve', 'tensor_copy', out=gst[:, :, 1:NCH], in_=G4[:, :, 0:NCH - 1, 63])
        op('dve', 'tensor_tensor', out=lam, in0=G4[:, :, :, 63], in1=gst, op=ALU.subtract)
        op('dve', 'tensor_tensor', out=mu, in0=G4[:, :, :, 63], in1=G4[:, :, :, 31], op=ALU.subtract)
        op('dve', 'tensor_tensor', out=nu, in0=G4[:, :, :, 31], in1=gst, op=ALU.subtract)
        for t in ('lam', 'mu', 'nu'):
            op('act', 'activation', out=A[t], in_=A[t], func=AF.Exp)
        op('dve', 'tensor_scalar', out=A['X1'], in0=A['X5'], scalar1=80.0, scalar2=None, op0=ALU.min)
        op('act', 'activation', out=A['X1'], in_=A['X1'], func=AF.Exp)
        op('dve', 'scalar_tensor_tensor', out=A['qt'], in0=A['X4'], scalar=128.0 ** -0.5, in1=A['X1'], op0=ALU.mult,
           op1=ALU.mult)
        op('dve', 'tensor_scalar', out=A['X4'], in0=A['X5'], scalar1=-1.0, scalar2=80.0, op0=ALU.mult, op1=ALU.min)
        op('act', 'activation', out=A['X4'], in_=A['X4'], func=AF.Exp)
        op('dve', 'tensor_tensor', out=A['kt'], in0=A['X2'], in1=A['X4'], op=ALU.mult)
        qt3 = A['qt'].re("p (h t) -> p h t", t=TT)
        kt3 = A['kt'].re("p (h t) -> p h t", t=TT)
        for gi in range(2):
            wt = ws.next("b_i%d" % gi)
            proj_tm(wt, 512, lambda b, ps, gi=gi: op('act', 'activation',
                                                     out=A['vT'][:, b * 1024 + gi * 512:b * 1024 + gi * 512 + 512],
                                                     in_=ps, func=AF.Copy))
        for gi in range(2):
            wt = ws.next("b_g%d" % gi)
            proj_tm(wt, 512, lambda b, ps, gi=gi: op('act', 'activation',
                                                     out=A['gs'][:, b * 1024 + gi * 512:b * 1024 + gi * 512 + 512],
                                                     in_=ps, func=AF.Silu))
        A2 = A
        qt3 = A2['qt'].re("p (h t) -> p h t", t=TT)
        kt3 = A2['kt'].re("p (h t) -> p h t", t=TT)
        lam = A2['lam'].re("p (h c) -> p h c", c=NCH)
        mu = A2['mu'].re("p (h c) -> p h c", c=NCH)
        nu = A2['nu'].re("p (h c) -> p h c", c=NCH)
        vT, gsv = A2['vT'], A2['gs']
        qa = A2['X1'].cast(BF16)[:, 0:8 * TT]
        qb = A2['X1'].cast(BF16)[:, 8 * TT:16 * TT]
        qa3 = qa.re("p (h t) -> p h t", t=TT)
        qb3 = qb.re("p (h t) -> p h t", t=TT)
        op('dve', 'tensor_tensor', out=qa3, in0=qt3, in1=evn.ubc(1, [128, 8, TT]), op=ALU.mult)
        op('dve', 'tensor_tensor', out=qb3, in0=qt3, in1=odd.ubc(1, [128, 8, TT]), op=ALU.mult)
        kTt = A2['X2'].cast(BF16)[:, 0:NB * 1024]
        At = A2['X2'].cast(BF16)[:, NB * 1024:2 * NB * 1024]
        ob = A2['X3'][:, 0:1024]
        AA = {'sq': A2['X3'][:, 1024:2048], 'yn': A2['X4'].cast(BF16)[:, 0:1024]}
        tmpu = A2['X5'][:, 0:1024]
        S = st_hg[l]
        S3 = S.re("p (h e) -> p h e", e=128)
        sb03 = sb0.re("p (h e) -> p h e", e=128)
        sb13 = sb1.re("p (h e) -> p h e", e=128)
        tmpu3 = tmpu.re("p (h e) -> p h e", e=128)
        dfr = [None]
        for b in range(NB):
            blk = slice(b * 128, (b + 1) * 128)
            pt = pb().cast(BF16)
            for h in range(8):
                op('pe', 'transpose', inc=(h == 7), out=pt[:, h * 128:(h + 1) * 128], in_=kt3[:, h, blk], identity=ident)
            kTb = kTt[:, b * 1024:(b + 1) * 1024]
            op('act', 'activation', out=kTb, in_=pt, func=AF.Copy)
            Atb = At[:, b * 1024:(b + 1) * 1024]
            At3 = Atb.re("p (h i) -> p h i", i=128)
            for hb in range(2):
                pa = pb()
                for i in range(4):
                    h = hb * 4 + i
                    op('pe', 'matmul', inc=(i == 3), out=pa[:, i * 128:(i + 1) * 128], lhsT=kt3[:, h, blk],
                       rhs=qt3[:, h, blk], start=True, stop=True)
                op('dve', 'tensor_tensor', out=At3[:, hb * 4:hb * 4 + 4, :], in0=pa.re("p (h i) -> p h i", i=128),
                   in1=hmask.ubc(1, [128, 4, 128]), op=ALU.mult)
                filler(1)
            vTb = vT[:, b * 1024:(b + 1) * 1024]
            for ci, sbx3 in ((0, sb03), (1, sb13)):
                c = 2 * b + ci
                rows = slice(ci * 64, ci * 64 + 64)
                op('dve', 'tensor_tensor', out=sbx3, in0=S3, in1=nu[:, :, c:c + 1].bc([128, 8, 128]), op=ALU.mult)
                for hb in range(2):
                    pu = pb()
                    for i in range(4):
                        h = hb * 4 + i
                        op('pe', 'matmul', inc=(i == 3), out=pu[:, i * 128:(i + 1) * 128],
                           lhsT=kTb[rows, h * 128:(h + 1) * 128], rhs=vTb[rows, h * 128:(h + 1) * 128], start=True,
                           stop=True)
                    hs = slice(hb * 4, hb * 4 + 4)
                    op('dve', 'tensor_tensor', out=tmpu3[:, hs, :], in0=pu.re("p (h e) -> p h e", e=128),
                       in1=mu[:, hs, c:c + 1].bc([128, 4, 128]), op=ALU.mult)
                    op('dve', 'tensor_tensor', out=S3[:, hs, :], in0=S3[:, hs, :],
                       in1=lam[:, hs, c:c + 1].bc([128, 4, 128]), op=ALU.mult)
                    op('dve', 'tensor_tensor', out=S3[:, hs, :], in0=S3[:, hs, :], in1=tmpu3[:, hs, :], op=ALU.add)
                    filler(1)
            for hb in range(2):
                po = pb()
                for i in range(4):
                    h = hb * 4 + i
                    o = po[:, i * 128:(i + 1) * 128]
                    op('pe', 'matmul', inc=False, out=o, lhsT=At3[:, h, :], rhs=vTb[:, h * 128:(h + 1) * 128],
                       start=True, stop=False)
                    op('pe', 'matmul', inc=False, out=o, lhsT=qa3[:, h, blk], rhs=sb03[:, h, :], start=False,
                       stop=False)
                    op('pe', 'matmul', inc=(i == 3), out=o, lhsT=qb3[:, h, blk], rhs=sb13[:, h, :], start=False,
                       stop=True)
                op('act', 'activation', out=ob[:, hb * 512:(hb + 1) * 512], in_=po, func=AF.Copy)
                filler(1)
            if dfr[0] is not None:
                dfr[0]()
            dfr[0] = post_norm_T(l, b, ob, 8, gsv[:, b * 1024:(b + 1) * 1024], C_HNRM, AA)
        dfr[0]()

    def mixer_lru(l):
        A = arena.carve([("xc", F32, 8 * TT), ("xcb", BF16, 8 * TT), ("hF", F32, 8 * TT)]
                        + [(nm + str(i), F32, TT) for i in range(2) for nm in ("r", "ig", "a", "a2", "u")]
                        + [("gg0", F32, TT), ("gg1", F32, TT)] + [("cin%d" % j, F32, TT + 3) for j in range(8)])
        cinj = [A["cin%d" % j] for j in range(8)]
        xc3 = A['xc'].re("p (c t) -> p c t", t=TT)
        xcb3 = A['xcb'].re("p (c t) -> p c t", t=TT)
        hF3 = A['hF'].re("p (c t) -> p c t", t=TT)
        lw = lruw[l].re("p (m n d) -> p m n d", m=2, n=8)
        tl3 = tail_lru[l].re("p (c t) -> p c t", t=3)
        for gi in range(2):
            wt = ws.next("d_x%d" % gi)

            def ev_x(j, ps, gi=gi):
                n = gi * 4 + j
                op('dve', 'tensor_copy', out=cinj[n][:, 0:3], in_=tl3[:, n, :])
                op('act', 'activation', out=cinj[n][:, 3:3 + TT], in_=ps, func=AF.Copy)
                op('dve', 'tensor_copy', out=tl3[:, n, :], in_=cinj[n][:, TT:TT + 3])
            proj_fm(wt, 512, ev_x)
        for n in range(8):
            conv_chunk(cinj[n], lambda t, n=n: colp[l][:, C_LCW + n * 4 + t:C_LCW + n * 4 + t + 1],
                       colp[l][:, C_LCB + n:C_LCB + n + 1], xc3[:, n, :])
            op('act', 'activation', out=xcb3[:, n, :], in_=xc3[:, n, :], func=AF.Copy)
            filler(1)
        for n in range(8):
            T = lambda nm, n=n: A[nm + str(n % 2)]
            pr = pb()
            op('pe', 'matmul', inc=False, out=pr[:, 0:TT], lhsT=lw[:, 0, n, :], rhs=xcb3[:, n, :], start=True, stop=True)
            op('pe', 'matmul', out=pr[:, TT:2 * TT], lhsT=lw[:, 1, n, :], rhs=xcb3[:, n, :], start=True, stop=True)
            op('act', 'activation', out=T('r'), in_=pr[:, 0:TT], func=AF.Sigmoid, bias=colp[l][:, C_LBA + n:C_LBA + n + 1])
            op('act', 'activation', out=T('ig'), in_=pr[:, TT:2 * TT], func=AF.Sigmoid,
               bias=colp[l][:, C_LBI + n:C_LBI + n + 1])
            op('act', 'activation', out=T('a'), in_=T('r'), func=AF.Exp, scale=lrup[l][:, n:n + 1])
            op('act', 'activation', out=T('a2'), in_=T('r'), func=AF.Exp, scale=lrup[l][:, 8 + n:9 + n])
            op('act', 'activation', out=T('a2'), in_=T('a2'), func=AF.Sqrt, scale=-1.0, bias=1.0)
            op('dve', 'tensor_tensor', out=T('u'), in0=xc3[:, n, :], in1=T('ig'), op=ALU.mult)
            op('dve', 'tensor_tensor', out=T('u'), in0=T('u'), in1=T('a2'), op=ALU.mult)
            op('dve', 'tensor_tensor_scan', out=hF3[:, n, :], data0=T('a'), data1=T('u'), initial=st_lru[l][:, n:n + 1],
               op0=ALU.mult, op1=ALU.add)
            op('dve', 'tensor_copy', out=st_lru[l][:, n:n + 1], in_=hF3[:, n, TT - 1:TT])
            filler(1)
        drain()
        for gi in range(2):
            wt = ws.next("d_g%d" % gi)

            def ev_g(j, ps, gi=gi):
                n = gi * 4 + j
                gg = A['gg%d' % (n % 2)]
                op('act', 'activation', out=gg, in_=ps, func=AF.Gelu_apprx_tanh)
                op('dve', 'tensor_tensor', out=ybr3s[cur['br'] % 2][:, n, :], in0=hF3[:, n, :], in1=gg, op=ALU.mult)
            proj_fm(wt, 512, ev_g)

    try:
      for t in range(ntiles):
          t0 = t * TT
          for b in range(NB):
              k.dma('sp', xs[b], x_d[t0 + b * 128:t0 + (b + 1) * 128, :])
          for l in range(nlayers):
              dbg = taps and t == 0 and l == 0
              rmsnorm_hT(l, C_MIXN)
              if dbg:
                  tap("hT", hT, [128, 16 * TT], BF16)
              ck('norm')
              cur['br'] = 0
              mixer_ssd(l)
              if dbg:
                  tap("ya", ybrs[cur['br'] % 2], [128, 8 * TT], BF16)
              ck('ssd')
              merge(l, 0)
              ck('merge0')
              cur['br'] = 1
              mixer_hgrn(l)
              drain()
              if dbg:
                  tap("yb", ybrs[cur['br'] % 2], [128, 8 * TT], BF16)
              ck('hgrn')
              merge(l, 1)
              cur['br'] = 2
              mixer_mlstm(l)
              drain()
              if dbg:
                  tap("yc", ybrs[cur['br'] % 2], [128, 8 * TT], BF16)
              ck('mlstm')
              merge(l, 2)
              cur['br'] = 3
              mixer_lru(l)
              if dbg:
                  tap("yd", ybrs[cur['br'] % 2], [128, 8 * TT], BF16)
              ck('lru')
              merge(l, 3, defer=False)
              if dbg:
                  tap("macc", macc, [128, 16 * TT])
              op('act', 'activation', out=hT, in_=macc, func=AF.Copy)
              for j in range(4):
                  w = w3(ws.next("wout_%d" % j), 16, 512)
                  for b in range(NB):
                      ps = pb()
                      for kc in range(16):
                          op('pe', 'matmul', inc=(kc == 15), out=ps, lhsT=hT3[:, kc, b * 128:(b + 1) * 128], rhs=w[:, kc, :],
                             start=(kc == 0), stop=(kc == 15))
                      xv = xs[b][:, j * 512:(j + 1) * 512]
                      op('dve', 'tensor_tensor', out=xv, in0=xv, in1=ps, op=ALU.add)
              if dbg:
                  tap("x1", xs[0], [128, D])
              ck('wout')
              rmsnorm_hT(l, C_MLPN)
              for q in range(4):
                  for j in range(4):
                      wt = ws.next("wup_%d" % (q * 4 + j))

                      def ev_up(c, ps, j=j):
                          r = sgb[c % 2][:, 0:TT]
                          op('act', 'activation', out=r, in_=ps, func=AF.Relu)
                          op('dve', 'tensor_tensor', out=aq3[:, j * 4 + c, :], in0=r, in1=r, op=ALU.mult)
                      proj_fm(wt, 512, ev_up)
                  for j in range(4):
                      w = w3(ws.next("wdn_%d_%d" % (q, j)), 16, 512)
                      for b in range(NB):
                          ps = pb()
                          for kc in range(16):
                              op('pe', 'matmul', inc=(kc == 15), out=ps, lhsT=aq3[:, kc, b * 128:(b + 1) * 128],
                                 rhs=w[:, kc, :], start=(kc == 0), stop=(kc == 15))
                          xv = xs[b][:, j * 512:(j + 1) * 512]
                          op('dve', 'tensor_tensor', out=xv, in0=xv, in1=ps, op=ALU.add)
              if dbg:
                  tap("x2", xs[0], [128, D])
              ck('mlp')
              rmsnorm_hT(l, C_PLEN)
              A = arena.carve([("pst", F32, 2 * TT), ("pb16", BF16, 2 * TT)])
              k.dma('sp', A['pst'].re("p (c t) -> p c t", t=TT),
                    pT_d[l].re("(c p) t -> p c t", p=128)[:, :, t0:t0 + TT])
              op('act', 'activation', out=A['pb16'], in_=A['pst'], func=AF.Copy)
              p3 = A['pb16'].re("p (c t) -> p c t", t=TT)
              for j in range(4):
                  wg = w3(ws.next("wpg_%d" % j), 16, 512)
                  wp = w3(ws.next("wple_%d" % j, keep_prev=True), 2, 512)
                  for b in range(NB):
                      pG = pb()
                      for kc in range(16):
                          op('pe', 'matmul', inc=(kc == 15), out=pG, lhsT=hT3[:, kc, b * 128:(b + 1) * 128], rhs=wg[:, kc, :],
                             start=(kc == 0), stop=(kc == 15))
                      pP = pb()
                      for kc in range(2):
                          op('pe', 'matmul', inc=(kc == 1), out=pP, lhsT=p3[:, kc, b * 128:(b + 1) * 128], rhs=wp[:, kc, :],
                             start=(kc == 0), stop=(kc == 1))
                      sg = sgb[(j * NB + b) % 2]
                      op('act', 'activation', out=sg, in_=pG, func=AF.Sigmoid)
                      op('dve', 'tensor_tensor', out=sg, in0=sg, in1=pP, op=ALU.mult)
                      xv = xs[b][:, j * 512:(j + 1) * 512]
                      op('dve', 'tensor_tensor', out=xv, in0=xv, in1=sg, op=ALU.add)
          A = arena.carve([("fn", F32, D), ("ob0", F32, D), ("ob1", F32, D)] + [("xn%d" % b, BF16, D) for b in range(NB)])
          xn = [A["xn%d" % b] for b in range(NB)]
          k.dma('sp', A['fn'], fnorm_d.m(lambda a: a.partition_broadcast(128)))
          for b in range(NB):
              ob = A['ob%d' % b]
              op('act', 'activation', out=xn[b], in_=xs[b], func=AF.Square, accum_out=ss[:, b:b + 1])
              op('act', 'activation', out=ss[:, 4 + b:5 + b], in_=ss[:, b:b + 1], func=AF.Sqrt, scale=1.0 / D, bias=EPS)
              op('dve', 'reciprocal', out=ss[:, 4 + b:5 + b], in_=ss[:, 4 + b:5 + b])
              op('dve', 'scalar_tensor_tensor', out=ob, in0=xs[b], scalar=ss[:, 4 + b:5 + b], in1=A['fn'], op0=ALU.mult,
                 op1=ALU.mult)
              k.dma('sp', out_d[t0 + b * 128:t0 + (b + 1) * 128, :], ob)
    except _Stop:
        pass
    k.finish()
    return nc, k


def _pack(W, groups, kc):
    parts = []
    for c0, n in groups:
        blk = W[:, c0:c0 + n].reshape(kc, 128, n).transpose(1, 0, 2).reshape(128, kc * n)
        parts.append(blk)
    return np.ascontiguousarray(np.concatenate(parts, axis=1))


def _col(v):
    return v.reshape(-1, 128).T


def prepare_weights(inp):
    L = 2
    f = lambda a: np.asarray(a, dtype=np.float32)
    g512 = lambda n: [(j * 512, 512) for j in range(n // 512)]
    win = np.stack([_pack(f(inp['w_in'][l]), [(c0, n) for _, c0, n in WIN_GROUPS], 16) for l in range(L)])
    wbr = np.stack([np.concatenate([_pack(f(inp['w_branch'][l, br]), g512(2048), 8) for br in range(4)], axis=1)
                    for l in range(L)])
    wout = np.stack([_pack(f(inp['w_out'][l]), g512(2048), 16) for l in range(L)])
    wup = np.stack([_pack(f(inp['w_up'][l]), g512(DFF), 16) for l in range(L)])
    wdn = np.stack([np.concatenate([_pack(f(inp['w_down'][l])[q * 2048:(q + 1) * 2048], g512(2048), 16)
                                    for q in range(4)], axis=1) for l in range(L)])
    wple = np.stack([_pack(f(inp['w_ple'][l]), g512(2048), 2) for l in range(L)])
    wpg = np.stack([_pack(f(inp['w_ple_gate'][l]), g512(2048), 16) for l in range(L)])
    lruw = np.stack([np.concatenate([f(inp['lru_wa'][l]).transpose(1, 0, 2).reshape(128, 1024),
                                     f(inp['lru_wi'][l]).transpose(1, 0, 2).reshape(128, 1024)], axis=1)
                     for l in range(L)])
    colp = np.zeros((L, 128, NCOL), np.float32)
    rowp = np.zeros((L, 1, NROW), np.float32)
    for l in range(L):
        c = colp[l]
        c[:, C_MIXN:C_MIXN + 16] = _col(f(inp['mix_norm'][l]))
        c[:, C_MLPN:C_MLPN + 16] = _col(f(inp['mlp_norm'][l]))
        c[:, C_PLEN:C_PLEN + 16] = _col(f(inp['ple_norm'][l]))
        c[:, C_SCW:C_SCW + 64] = f(inp['ssm_conv_w'][l]).T.reshape(16, 128, 4).transpose(1, 0, 2).reshape(128, 64)
        c[:, C_SCB:C_SCB + 16] = _col(f(inp['ssm_conv_b'][l]))
        c[:, C_SNRM:C_SNRM + 8] = _col(f(inp['ssm_norm'][l]))
        c[:, C_HNRM:C_HNRM + 8] = _col(f(inp['hgrn_norm'][l]))
        c[:, C_MNRM:C_MNRM + 8] = _col(f(inp['mlstm_norm'][l]))
        c[:, C_LCW:C_LCW + 32] = f(inp['lru_conv_w'][l]).T.reshape(8, 128, 4).transpose(1, 0, 2).reshape(128, 32)
        c[:, C_LCB:C_LCB + 8] = _col(f(inp['lru_conv_b'][l]))
        c[:, C_LBA:C_LBA + 8] = _col(f(inp['lru_ba'][l]))
        c[:, C_LBI:C_LBI + 8] = _col(f(inp['lru_bi'][l]))
        c[:, C_LAP:C_LAP + 8] = _col(f(inp['lru_a_param'][l]))
        c[:, C_LB0:C_LB0 + 8] = _col(f(inp['hgrn_lb_logits'][0]))
        c[:, C_LB1:C_LB1 + 8] = _col(f(inp['hgrn_lb_logits'][1]))
        r = rowp[l, 0]
        r[R_DTB:R_DTB + 16] = f(inp['ssm_dt_bias'][l])
        r[R_ALOG:R_ALOG + 16] = f(inp['ssm_a_log'][l])
        r[R_DSK:R_DSK + 16] = f(inp['ssm_d'][l])
        r[R_IFB:R_IFB + 4] = f(inp['mlstm_i_bias'][l])
        r[R_IFB + 4:R_IFB + 8] = f(inp['mlstm_f_bias'][l])
    return dict(win=win, wbr=wbr, wout=wout, wup=wup, wdn=wdn, wple=wple, wpg=wpg, lruw=np.ascontiguousarray(lruw),
                colp=colp, rowp=rowp, fnorm=f(inp['final_norm']).reshape(1, D))


def kernel(**inputs):
    x = np.asarray(inputs['x'], dtype=np.float32)
    p = np.asarray(inputs['p'], dtype=np.float32)
    wts = prepare_weights(inputs)
    nc, _ = build()
    in_maps = []
    for b in range(8):
        m = dict(wts)
        m['x'] = np.ascontiguousarray(x[b])
        m['pT'] = np.ascontiguousarray(p[:, b].transpose(0, 2, 1))
        in_maps.append(m)
    res = run_bass_kernel_spmd(nc, in_maps, core_ids=list(range(8)))
    return np.stack([r['out'] for r in res.results], axis=0)
```

```python
import math
import numpy as np
import concourse.bass as bass
import concourse.mybir as mybir
from concourse.bass_utils import run_bass_kernel_spmd

F32 = mybir.dt.float32
BF16 = mybir.dt.bfloat16
AF = mybir.ActivationFunctionType
ALU = mybir.AluOpType
AX = mybir.AxisListType

D = 2048
SEQ = 4096
DIN = 20504
DFF = 8192
TT = 256
REV = False
XTE = 'act'
NB = TT // 128
EPS = 1e-6

WIN_GROUPS = [
    ("a_z0", 0, 512), ("a_z1", 512, 512), ("a_dt", 3072, 16),
    ("a_x0", 1024, 512), ("a_x1", 1536, 512), ("a_B", 2048, 512), ("a_C", 2560, 512),
    ("b_q0", 3088, 512), ("b_q1", 3600, 512), ("b_f0", 4112, 512), ("b_f1", 4624, 512),
    ("b_i0", 5136, 512), ("b_i1", 5648, 512), ("b_g0", 6160, 512), ("b_g1", 6672, 512),
    ("c_if", 10256, 8), ("c_q", 7184, 512), ("c_k", 7696, 512), ("c_v0", 8208, 512), ("c_v1", 8720, 512),
    ("c_o0", 9232, 512), ("c_o1", 9744, 512),
    ("d_x0", 10264, 512), ("d_x1", 10776, 512), ("d_g0", 11288, 512), ("d_g1", 11800, 512),
] + [("gate%d_%d" % (br, j), 12312 + br * 2048 + j * 512, 512) for br in range(4) for j in range(4)]
WIN_OFF = {}
_o = 0
for _n, _c0, _w in WIN_GROUPS:
    WIN_OFF[_n] = (_o, _w)
    _o += 16 * _w
WIN_TOT = _o

C_MIXN, C_MLPN, C_PLEN, C_SCW, C_SCB, C_SNRM, C_HNRM, C_MNRM = 0, 16, 32, 48, 112, 128, 136, 144
C_LCW, C_LCB, C_LBA, C_LBI, C_LAP, C_LB0, C_LB1, NCOL = 152, 184, 192, 200, 208, 216, 224, 232
R_DTB, R_ALOG, R_DSK, R_IFB, NROW = 0, 16, 32, 48, 56


class _Stop(Exception):
    pass


class Buf:
    __slots__ = ("name", "w", "r", "psum")

    def __init__(self, name):
        self.name = name
        self.w = None
        self.r = {}
        self.psum = False


class V:
    __slots__ = ("ap", "buf")

    def __init__(self, ap, buf):
        self.ap = ap
        self.buf = buf

    def __getitem__(self, idx):
        return V(self.ap[idx], self.buf)

    def m(self, fn):
        return V(fn(self.ap), self.buf)

    def bc(self, shape):
        return V(self.ap.to_broadcast(list(shape)), self.buf)

    def ubc(self, axis, shape):
        return V(self.ap.unsqueeze(axis).to_broadcast(list(shape)), self.buf)

    def re(self, s, **kw):
        return V(self.ap.rearrange(s, **kw), self.buf)

    def cast(self, dt):
        return V(self.ap.bitcast(dt), self.buf)


def _merge(d, tok):
    sid = id(tok[0])
    o = d.get(sid)
    if o is None or o[1] < tok[1]:
        d[sid] = tok


class K:
    NDMA = 6

    def __init__(self, nc):
        self.nc = nc
        self.eng = {'pe': nc.tensor, 'act': nc.scalar, 'dve': nc.vector, 'pool': nc.gpsimd, 'sp': nc.sync}
        self.sem = {e: nc.alloc_semaphore(name="sem_" + e) for e in self.eng}
        self.cnt = {e: 0 for e in self.eng}
        self.waited = {e: {} for e in self.eng}
        self.dsem = {}
        self.dcnt = {}
        self.dnext = {}
        for q in ('sp', 'pool'):
            self.dsem[q] = [nc.alloc_semaphore(name="dma_%s%d" % (q, i)) for i in range(self.NDMA)]
            self.dcnt[q] = [0] * self.NDMA
            self.dnext[q] = 0
        self.ninstr = 0
        self.limit = None
        self.streams = {e: [] for e in self.eng}

    def sb(self, name, shape, dt):
        t = self.nc.alloc_sbuf_tensor(name, list(shape), dt)
        return V(t[tuple(slice(None) for _ in shape)], Buf(name))

    def ps(self, name, shape, dt):
        t = self.nc.alloc_psum_tensor(name, list(shape), dt)
        b = Buf(name)
        b.psum = True
        return V(t[tuple(slice(None) for _ in shape)], b)

    def dram(self, name, shape, dt, kind="Internal"):
        t = self.nc.dram_tensor(name, list(shape), dt, kind=kind)
        return V(t.ap(), Buf(name))

    def _wait(self, e, tok):
        sem, val = tok
        key = id(sem)
        if self.waited[e].get(key, 0) >= val:
            return
        self.eng[e].wait_ge(sem, val)
        self.streams[e].append(('w', key, val))
        self.waited[e][key] = val
        self.ninstr += 1

    def _deps(self, e, reads, writes, skip_own=False):
        own = id(self.sem[e])
        for b in reads:
            if b.w is not None and not (skip_own and id(b.w[0]) == own):
                self._wait(e, b.w)
            if b.psum:
                for sid, tok in b.r.items():
                    if sid != own:
                        self._wait(e, tok)
        for b in writes:
            if b.w is not None and not (skip_own and id(b.w[0]) == own):
                self._wait(e, b.w)
            for sid, tok in b.r.items():
                if skip_own and sid == own:
                    continue
                self._wait(e, tok)

    def _commit(self, tok, reads, writes):
        for b in reads:
            _merge(b.r, tok)
        for b in writes:
            b.w = tok
            b.r = {}

    def op(self, e, name, inc=True, **kw):
        reads = []
        writes = []
        args = {}
        for key, v in kw.items():
            if isinstance(v, V):
                (writes if key in ('out', 'accum_out', 'ap') else reads).append(v.buf)
                args[key] = v.ap
            else:
                args[key] = v
        if self.limit is not None and self.ninstr >= self.limit:
            raise _Stop()
        self._deps(e, reads, writes, skip_own=(e == 'pe'))
        ins = getattr(self.eng[e], name)(**args)
        self.ninstr += 1
        if inc:
            self.cnt[e] += 1
            ins.then_inc(self.sem[e], 1)
            self.streams[e].append(('i', id(self.sem[e]), 1))
            tok = (self.sem[e], self.cnt[e])
        else:
            tok = (self.sem[e], self.cnt[e] + 1)
        self._commit(tok, reads, writes)
        return ins

    def dma(self, q, out, in_, **kw):
        s = self.dnext[q]
        self.dnext[q] = (s + 1) % self.NDMA
        sem = self.dsem[q][s]
        if self.dcnt[q][s] > 0:
            self._wait(q, (sem, 16 * self.dcnt[q][s]))
        self._deps(q, [in_.buf], [out.buf])
        ins = self.eng[q].dma_start(out=out.ap, in_=in_.ap, **kw)
        self.ninstr += 1
        self.dcnt[q][s] += 1
        ins.then_inc(sem, 16)
        self.streams[q].append(('i', id(sem), 16))
        tok = (sem, 16 * self.dcnt[q][s])
        self._commit(tok, [in_.buf], [out.buf])
        return tok

    def finish(self):
        for q in self.dsem:
            for i, sem in enumerate(self.dsem[q]):
                if self.dcnt[q][i] > 0:
                    self._wait('sp', (sem, 16 * self.dcnt[q][i]))


def check_deadlock(k):
    pos = {e: 0 for e in k.streams}
    val = {}
    progress = True
    while progress:
        progress = False
        for e, st in k.streams.items():
            while pos[e] < len(st):
                kind, sid, v = st[pos[e]]
                if kind == 'w':
                    if val.get(sid, 0) >= v:
                        pos[e] += 1
                        progress = True
                    else:
                        break
                else:
                    val[sid] = val.get(sid, 0) + v
                    pos[e] += 1
                    progress = True
    stuck = {e: (pos[e], len(st)) for e, st in k.streams.items() if pos[e] < len(st)}
    return stuck


class Arena:
    def __init__(self, k, name, nwords):
        self.k = k
        self.base = k.sb(name, [128, nwords], F32)
        self.nwords = nwords
        self.live = [self.base.buf]

    def carve(self, specs):
        toks = {}
        for b in self.live:
            if b.w is not None:
                _merge(toks, b.w)
            for t in b.r.values():
                _merge(toks, t)
        self.live = []
        out = {}
        off = 0
        for name, dt, n in specs:
            words = n if dt == F32 else (n + 1) // 2
            assert off + words <= self.nwords, (name, off, words, self.nwords)
            ap = self.base.ap[:, off:off + words]
            if dt != F32:
                ap = ap.bitcast(dt)[:, 0:n]
            b = Buf(name)
            b.r = dict(toks)
            self.live.append(b)
            out[name] = V(ap, b)
            off += words
        return out


class WStream:
    def __init__(self, k, nslots, slot_elems):
        self.k = k
        self.slots = [k.sb("wt%d" % i, [128, slot_elems], BF16) for i in range(nslots)]
        self.n = nslots
        self.plan = []
        self.tok = {}
        self.issued = 0
        self.cur = 0

    def _issue(self, i):
        name, src, scr, off, ln, first, key = self.plan[i]
        slot = self.slots[i % self.n][:, 0:ln]
        if first:
            self.k.dma('pool', slot, src[:, off:off + ln])
            self.tok[key] = self.k.dma('sp', V(scr.ap[:, off:off + ln], Buf("wscr_w")), slot)
        else:
            self.k._wait('sp', self.tok[key])
            self.k.dma('sp', slot, V(scr.ap[:, off:off + ln], Buf("wscr")))

    def next(self, name, keep_prev=False):
        i = self.cur
        lim = min(len(self.plan), i + self.n - (1 if keep_prev else 0))
        while self.issued < lim:
            self._issue(self.issued)
            self.issued += 1
        assert self.plan[i][0] == name, (self.plan[i][0], name)
        self.cur += 1
        return self.slots[i % self.n]


def layer_plan(wd, l):
    P = []

    def win(nm):
        o, w = WIN_OFF[nm]
        P.append((nm, wd['win'][l], o, 16 * w))

    def merge(br):
        for j in range(4):
            win("gate%d_%d" % (br, j))
            P.append(("wbr%d_%d" % (br, j), wd['wbr'][l], (br * 4 + j) * 8 * 512, 8 * 512))

    for nm in ("a_dt", "a_x0", "a_x1", "a_z0", "a_B", "a_C", "a_z1"):
        win(nm)
    for nm in ("b_q0", "b_q1", "b_f0", "b_f1", "b_i0", "b_i1", "b_g0", "b_g1"):
        win(nm)
    merge(0)
    for nm in ("c_if", "c_q", "c_k", "c_v0", "c_v1", "c_o0", "c_o1"):
        win(nm)
    merge(1)
    for nm in ("d_x0", "d_x1"):
        win(nm)
    merge(2)
    for nm in ("d_g0", "d_g1"):
        win(nm)
    merge(3)
    for j in range(4):
        P.append(("wout_%d" % j, wd['wout'][l], j * 16 * 512, 16 * 512))
    for q in range(4):
        for j in range(4):
            P.append(("wup_%d" % (q * 4 + j), wd['wup'][l], (q * 4 + j) * 16 * 512, 16 * 512))
        for j in range(4):
            P.append(("wdn_%d_%d" % (q, j), wd['wdn'][l], (q * 4 + j) * 16 * 512, 16 * 512))
    for j in range(4):
        P.append(("wpg_%d" % j, wd['wpg'][l], j * 16 * 512, 16 * 512))
        P.append(("wple_%d" % j, wd['wple'][l], j * 2 * 512, 2 * 512))
    return P


def build(ntiles=SEQ // TT, nlayers=2, taps=False, stop=None, limit=None):
    nc = bass.Bass("TRN2", target_bir_lowering=False)
    k = K(nc)
    k.limit = limit
    op = k.op
    ntok = ntiles * TT

    x_d = k.dram("x", [SEQ, D], F32, "ExternalInput")
    pT_d = k.dram("pT", [2, 256, SEQ], F32, "ExternalInput")
    wd = {
        'win': k.dram("win", [2, 128, WIN_TOT], F32, "ExternalInput"),
        'wbr': k.dram("wbr", [2, 128, 4 * 8 * 2048], F32, "ExternalInput"),
        'wout': k.dram("wout", [2, 128, 16 * 2048], F32, "ExternalInput"),
        'wup': k.dram("wup", [2, 128, 16 * DFF], F32, "ExternalInput"),
        'wdn': k.dram("wdn", [2, 128, 64 * 2048], F32, "ExternalInput"),
        'wple': k.dram("wple", [2, 128, 2 * 2048], F32, "ExternalInput"),
        'wpg': k.dram("wpg", [2, 128, 16 * 2048], F32, "ExternalInput"),
    }
    lruw_d = k.dram("lruw", [2, 128, 2 * 8 * 128], F32, "ExternalInput")
    colp_d = k.dram("colp", [2, 128, NCOL], F32, "ExternalInput")
    rowp_d = k.dram("rowp", [2, 1, NROW], F32, "ExternalInput")
    fnorm_d = k.dram("fnorm", [1, D], F32, "ExternalInput")
    out_d = k.dram("out", [SEQ, D], F32, "ExternalOutput")
    tap_outs = []

    def tap(name, v, shape, dt=F32):
        if not taps:
            return
        t = k.dram("tap_" + name, list(shape), dt, "ExternalOutput")
        k.dma('sp', t, v)
        tap_outs.append(t)

    xs = [k.sb("xs%d" % b, [128, D], F32) for b in range(NB)]
    hT = k.sb("hT", [128, 16 * TT], BF16)
    hT3 = hT.re("p (c t) -> p c t", t=TT)
    macc = k.sb("macc", [128, 16 * TT], F32)
    macc3 = macc.re("p (c t) -> p c t", t=TT)
    aq = macc.cast(BF16)[:, 0:16 * TT]
    aq3 = aq.re("p (c t) -> p c t", t=TT)
    ybrs = [k.sb("ybr%d" % i, [128, 8 * TT], BF16) for i in range(2)]
    ybr3s = [y.re("p (c t) -> p c t", t=TT) for y in ybrs]
    cur = {'br': 0}
    pending = {'gen': None}

    def filler(n=1):
        for _ in range(n):
            g = pending['gen']
            if g is None:
                return
            try:
                next(g)
            except StopIteration:
                pending['gen'] = None

    def drain():
        while pending['gen'] is not None:
            filler()
    ws = WStream(k, 3, 16 * 512)
    colp = [k.sb("colp%d" % l, [128, NCOL], F32) for l in range(2)]
    rowp = [k.sb("rowp%d" % l, [128, NROW], F32) for l in range(2)]
    arow = [k.sb("arow%d" % l, [128, 16], F32) for l in range(2)]
    lbp = [k.sb("lbp%d" % l, [128, 16], F32) for l in range(2)]
    lrup = [k.sb("lrup%d" % l, [128, 16], F32) for l in range(2)]
    lruw = [k.sb("lruw%d" % l, [128, 2 * 8 * 128], BF16) for l in range(2)]
    ident = k.sb("ident", [128, 128], BF16)
    identf = k.sb("identf", [128, 128], F32)
    Umat = k.sb("Umat", [128, 128], F32)
    negU = k.sb("negU", [128, 128], F32)
    NEGM = k.sb("NEGM", [128, 128], F32)
    ones = k.sb("ones", [128, TT], F32)
    hmask = k.sb("hmask", [128, 128], F32)
    evn = k.sb("evn", [128, TT], BF16)
    odd = k.sb("odd", [128, TT], BF16)
    sm = k.sb("sm", [128, 512], F32)
    sm2 = k.sb("sm2", [128, 256], F32)
    sm_ecs = k.sb("sm_ecs", [128, 16], F32)
    sm_dst = k.sb("sm_dst", [128, 16], F32)
    sm_etot = k.sb("sm_etot", [128, 16], F32)
    ss = k.sb("ss", [128, 8], F32)
    hgp = k.sb("hgp", [128, 128], F32)
    st_ssd = [k.sb("st_ssd%d" % l, [128, 1024], F32) for l in range(2)]
    st_ml = [k.sb("st_ml%d" % l, [128, 4 * 257], F32) for l in range(2)]
    st_hg = [k.sb("st_hg%d" % l, [128, 1024], F32) for l in range(2)]
    st_lru = [k.sb("st_lru%d" % l, [128, 8], F32) for l in range(2)]
    tail_ssd = [k.sb("tail_ssd%d" % l, [128, 16 * 3], F32) for l in range(2)]
    tail_lru = [k.sb("tail_lru%d" % l, [128, 8 * 3], F32) for l in range(2)]
    stb = k.sb("stb", [128, 4 * 257 + 4], BF16)
    sb0 = k.sb("sb0", [128, 1024], BF16)
    sb1 = k.sb("sb1", [128, 1024], BF16)
    CINW = 8 * (TT + 3)
    arena = Arena(k, "arena", 14400)
    banks = [k.ps("bank%d" % i, [128, 512], F32) for i in range(8)]
    bstate = {'i': 0}

    def pb():
        b = banks[bstate['i'] % 8]
        bstate['i'] += 1
        return b

    op('dve', 'memset', ap=identf, constant=1.0)
    op('pool', 'affine_select', out=identf, in_=identf, pattern=[[-1, 128]], compare_op=ALU.is_equal, fill=0.0,
       base=0, channel_multiplier=1)
    op('dve', 'tensor_copy', out=ident, in_=identf)
    op('dve', 'memset', ap=Umat, constant=1.0)
    op('pool', 'affine_select', out=Umat, in_=Umat, pattern=[[1, 128]], compare_op=ALU.is_ge, fill=0.0,
       base=0, channel_multiplier=-1)
    op('dve', 'tensor_scalar', out=negU, in0=Umat, scalar1=-1.0, scalar2=None, op0=ALU.mult)
    op('dve', 'memset', ap=NEGM, constant=-30000.0)
    op('pool', 'affine_select', out=NEGM, in_=NEGM, pattern=[[-1, 128]], compare_op=ALU.is_gt, fill=0.0,
       base=0, channel_multiplier=1)
    op('dve', 'memset', ap=ones, constant=1.0)
    op('dve', 'tensor_copy', out=hmask, in_=Umat)
    op('dve', 'memset', ap=hmask[0:64, 64:128], constant=0.0)
    op('dve', 'memset', ap=evn, constant=1.0)
    op('dve', 'memset', ap=evn.re("p (a two c) -> p a two c", two=2, c=64)[:, :, 1, :], constant=0.0)
    op('dve', 'memset', ap=odd, constant=0.0)
    op('dve', 'memset', ap=odd.re("p (a two c) -> p a two c", two=2, c=64)[:, :, 1, :], constant=1.0)
    for l in range(2):
        for t in (st_ssd[l], st_ml[l], st_hg[l], st_lru[l], tail_ssd[l], tail_lru[l]):
            op('dve', 'memset', ap=t, constant=0.0)
        k.dma('sp', colp[l], colp_d[l])
        k.dma('sp', rowp[l], rowp_d[l].m(lambda a: a.partition_broadcast(128)))
        k.dma('pool', lruw[l], lruw_d[l])
        op('act', 'activation', out=arow[l], in_=rowp[l][:, R_ALOG:R_ALOG + 16], func=AF.Exp)
        op('dve', 'tensor_scalar', out=arow[l], in0=arow[l], scalar1=-1.0, scalar2=None, op0=ALU.mult)
        op('act', 'activation', out=lrup[l][:, 0:8], in_=colp[l][:, C_LAP:C_LAP + 8], func=AF.Exp, scale=-1.0)
        op('act', 'activation', out=lrup[l][:, 0:8], in_=lrup[l][:, 0:8], func=AF.Ln, bias=1.0)
        op('dve', 'tensor_scalar', out=lrup[l][:, 8:16], in0=lrup[l][:, 0:8], scalar1=-16.0, scalar2=None, op0=ALU.mult)
        op('dve', 'tensor_scalar', out=lrup[l][:, 0:8], in0=lrup[l][:, 0:8], scalar1=-8.0, scalar2=None, op0=ALU.mult)
    op('dve', 'memset', ap=lbp[0][:, 0:8], constant=0.0)
    op('dve', 'memset', ap=lbp[0][:, 8:16], constant=1.0)
    op('dve', 'tensor_tensor', out=lbp[1][:, 0:8], in0=colp[0][:, C_LB0:C_LB0 + 8], in1=colp[0][:, C_LB1:C_LB1 + 8],
       op=ALU.subtract)
    op('act', 'activation', out=lbp[1][:, 0:8], in_=lbp[1][:, 0:8], func=AF.Sigmoid)
    op('dve', 'tensor_scalar', out=lbp[1][:, 8:16], in0=lbp[1][:, 0:8], scalar1=-1.0, scalar2=1.0, op0=ALU.mult,
       op1=ALU.add)

    wscr = {nm: k.dram(nm + "_bf16", [2, 128, v.ap.shape[2]], BF16) for nm, v in wd.items()}
    first_plan = {}
    for l in range(nlayers):
        ents = []
        for name, src, off, ln in layer_plan(wd, l):
            key = [nm for nm, v in wd.items() if v.buf is src.buf][0]
            ents.append((name, key, off, ln))
        first_plan[l] = ents
    plan = []
    for t in range(ntiles):
        for l in range(nlayers):
            for name, key, off, ln in first_plan[l]:
                plan.append((name, wd[key][l], wscr[key][l], off, ln, t == 0, (l, name)))
    ws.plan = plan

    def ck(name):
        if stop == name:
            raise _Stop()

    def w3(wt, kc, n):
        return wt[:, 0:kc * n].re("p (k c) -> p k c", c=n)

    def proj_fm(wt, n, evac, rhs3=None, kc=16):
        w = w3(wt, kc, n)
        rhs3 = hT3 if rhs3 is None else rhs3
        for j in range(n // 128):
            ps = pb()
            for c in range(kc):
                op('pe', 'matmul', inc=(c == kc - 1), out=ps[:, 0:TT], lhsT=w[:, c, j * 128:(j + 1) * 128],
                   rhs=rhs3[:, c, :], start=(c == 0), stop=(c == kc - 1))
            evac(j, ps[:, 0:TT])

    def proj_tm(wt, n, evac, lhs3=None, kc=16):
        w = w3(wt, kc, n)
        lhs3 = hT3 if lhs3 is None else lhs3
        for b in range(NB):
            ps = pb()
            for c in range(kc):
                op('pe', 'matmul', inc=(c == kc - 1), out=ps[:, 0:n], lhsT=lhs3[:, c, b * 128:(b + 1) * 128],
                   rhs=w[:, c, :], start=(c == 0), stop=(c == kc - 1))
            evac(b, ps[:, 0:n])

    def rmsnorm_hT(l, coff):
        A = arena.carve([("xn%d" % b, BF16, D) for b in range(NB)])
        xn = [A["xn%d" % b] for b in range(NB)]
        for b in range(NB):
            op('act', 'activation', out=xn[b], in_=xs[b], func=AF.Square, accum_out=ss[:, b:b + 1])
            op('act', 'activation', out=ss[:, 4 + b:5 + b], in_=ss[:, b:b + 1], func=AF.Sqrt, scale=1.0 / D, bias=EPS)
            op('dve', 'reciprocal', out=ss[:, 4 + b:5 + b], in_=ss[:, 4 + b:5 + b])
            op('dve', 'tensor_scalar', out=xn[b], in0=xs[b], scalar1=ss[:, 4 + b:5 + b], scalar2=None, op0=ALU.mult)
            for half in range(2):
                pt = pb().cast(BF16)
                for c in range(8):
                    cc = half * 8 + c
                    op('pe', 'transpose', inc=(c == 7), out=pt[:, c * 128:(c + 1) * 128],
                       in_=xn[b][:, cc * 128:(cc + 1) * 128], identity=ident)
                op('dve', 'tensor_tensor', out=hT3[:, half * 8:half * 8 + 8, b * 128:(b + 1) * 128],
                   in0=pt.re("p (c t) -> p c t", t=128),
                   in1=colp[l][:, coff + half * 8:coff + half * 8 + 8].ubc(2, [128, 8, 128]), op=ALU.mult)

    def conv_chunk(src, wcol, bcol, acc):
        op('dve', 'tensor_scalar', out=acc, in0=src[:, 0:TT], scalar1=wcol(0), scalar2=bcol, op0=ALU.mult,
           op1=ALU.add)
        for t in range(1, 4):
            op('dve', 'scalar_tensor_tensor', out=acc, in0=src[:, t:t + TT], scalar=wcol(t), in1=acc,
               op0=ALU.mult, op1=ALU.add)

    def lin_attn_chunk(H, G, P, adt, qF, kF, kT, vT2, st2, stb2, y2, A):
        R = H // G
        v3 = lambda v: v.re("p (h q) -> p h q", q=P)
        pc = pb()
        op('pe', 'matmul', inc=False, out=pc[:, 0:H], lhsT=Umat, rhs=adt, start=True, stop=True)
        op('pe', 'matmul', out=pc[:, H:2 * H], lhsT=ones[:, 0:128], rhs=adt, start=True, stop=True)
        csb = sm[:, 0:2 * H]
        op('dve', 'tensor_copy', out=csb, in_=pc[:, 0:2 * H])
        cs, tot = sm[:, 0:H], sm[:, H:2 * H]
        ecs, dst, etot = sm_ecs[:, 0:H], sm_dst[:, 0:H], sm_etot[:, 0:H]
        op('act', 'activation', out=ecs, in_=cs, func=AF.Exp)
        op('dve', 'tensor_tensor', out=dst, in0=tot, in1=cs, op=ALU.subtract)
        op('act', 'activation', out=dst, in_=dst, func=AF.Exp)
        op('act', 'activation', out=etot, in_=tot, func=AF.Exp)
        vw = A['vw'][:, 0:H * P]
        op('dve', 'tensor_tensor', out=v3(vw), in0=v3(vT2), in1=dst.ubc(2, [128, H, P]), op=ALU.mult)
        pg = pb()
        for g in range(G):
            op('pe', 'matmul', inc=(g == G - 1), out=pg[:, g * 128:(g + 1) * 128], lhsT=kF(g), rhs=qF(g), start=True,
               stop=True)
        MT = A['MT']
        MT3 = MT.re("p (h t) -> p h t", t=128)
        for hb in range(H // 4):
            pd = pb()
            for i in range(4):
                h = hb * 4 + i
                o = pd[:, i * 128:(i + 1) * 128]
                op('pe', 'matmul', inc=False, out=o, lhsT=adt[:, h:h + 1].bc([128, 128]), rhs=Umat, start=True,
                   stop=False)
                op('pe', 'matmul', inc=False, out=o, lhsT=negU, rhs=adt[:, h:h + 1].bc([128, 128]), start=False,
                   stop=False)
                op('pe', 'matmul', inc=(i == 3), out=o, lhsT=identf, rhs=NEGM, start=False, stop=True)
            E = A['E%d' % (hb % 2)]
            op('act', 'activation', out=E, in_=pd, func=AF.Exp)
            if R == 4:
                gin = pg[:, hb * 128:(hb + 1) * 128].ubc(1, [128, 4, 128])
            else:
                gin = pg.re("p (h t) -> p h t", t=128)
            op('dve', 'tensor_tensor', out=MT3[:, hb * 4:hb * 4 + 4, :], in0=E.re("p (h t) -> p h t", t=128), in1=gin,
               op=ALU.mult)
            filler(1)
        HPB = max(1, 512 // P)
        for bk in range((H + HPB - 1) // HPB):
            hs = list(range(bk * HPB, min(H, (bk + 1) * HPB)))
            n = len(hs)
            pyd = pb()
            pyo = pb()
            for idx, h in enumerate(hs):
                op('pe', 'matmul', inc=(idx == n - 1), out=pyd[:, idx * P:(idx + 1) * P], lhsT=MT3[:, h, :],
                   rhs=vT2[:, h * P:(h + 1) * P], start=True, stop=True)
            for idx, h in enumerate(hs):
                op('pe', 'matmul', inc=(idx == n - 1), out=pyo[:, idx * P:(idx + 1) * P], lhsT=qF(h // R),
                   rhs=stb2[:, h * P:(h + 1) * P], start=True, stop=True)
            yv = v3(y2)[:, hs[0]:hs[0] + n, :]
            op('dve', 'tensor_tensor', out=yv, in0=pyo[:, 0:n * P].re("p (h q) -> p h q", q=P),
               in1=ecs[:, hs[0]:hs[0] + n].ubc(2, [128, n, P]), op=ALU.mult)
            op('dve', 'tensor_tensor', out=yv, in0=yv, in1=pyd[:, 0:n * P].re("p (h q) -> p h q", q=P), op=ALU.add)
            filler(1)
        GPB = max(1, 512 // (R * P))
        for bk in range((G + GPB - 1) // GPB):
            gs = list(range(bk * GPB, min(G, (bk + 1) * GPB)))
            pu = pb()
            for idx, g in enumerate(gs):
                op('pe', 'matmul', inc=(idx == len(gs) - 1), out=pu[:, idx * R * P:(idx + 1) * R * P], lhsT=kT(g),
                   rhs=vw[:, g * R * P:(g + 1) * R * P], start=True, stop=True)
            h0, nh = gs[0] * R, len(gs) * R
            sv = v3(st2)[:, h0:h0 + nh, :]
            op('dve', 'tensor_tensor', out=sv, in0=sv, in1=etot[:, h0:h0 + nh].ubc(2, [128, nh, P]), op=ALU.mult)
            op('dve', 'tensor_tensor', out=sv, in0=sv, in1=pu[:, 0:nh * P].re("p (h q) -> p h q", q=P), op=ALU.add)
            op('act', 'activation', out=v3(stb2)[:, h0:h0 + nh, :], in_=sv, func=AF.Copy)
            filler(1)

    def post_norm_T(l, b, y, ng, gate_after, coff, A):
        gsz = 1024 // ng
        sq = A['sq']
        y3 = y.re("p (g c) -> p g c", g=ng)
        op('dve', 'tensor_tensor', out=sq, in0=y, in1=y, op=ALU.mult)
        sg = sm2[:, 0:ng]
        op('dve', 'tensor_reduce', out=sg, in_=sq.re("p (g c) -> p g c", g=ng), axis=AX.X, op=ALU.add)
        op('act', 'activation', out=sg, in_=sg, func=AF.Sqrt, scale=1.0 / gsz, bias=EPS)
        op('dve', 'reciprocal', out=sg, in_=sg)
        yn = A['yn']
        if gate_after is None:
            op('dve', 'tensor_tensor', out=yn.re("p (g c) -> p g c", g=ng), in0=y3, in1=sg.ubc(2, [128, ng, gsz]),
               op=ALU.mult)
        else:
            op('dve', 'tensor_tensor', out=y3, in0=y3, in1=sg.ubc(2, [128, ng, gsz]), op=ALU.mult)
            op('dve', 'tensor_tensor', out=yn, in0=y, in1=gate_after, op=ALU.mult)
        br_now = cur['br']

        def part2():
            pt = pb().cast(BF16)
            for c in range(8):
                op('pe', 'transpose', inc=(c == 7), out=pt[:, c * 128:(c + 1) * 128], in_=yn[:, c * 128:(c + 1) * 128],
                   identity=ident)
            op('dve', 'tensor_tensor', out=ybr3s[br_now % 2][:, :, b * 128:(b + 1) * 128],
               in0=pt.re("p (c t) -> p c t", t=128), in1=colp[l][:, coff:coff + 8].ubc(2, [128, 8, 128]), op=ALU.mult)
        return part2

    def merge_gen(l, br):
        yb3 = ybr3s[br % 2]
        for j in range(4):
            wg = w3(ws.next("gate%d_%d" % (br, j)), 16, 512)
            wb = w3(ws.next("wbr%d_%d" % (br, j), keep_prev=True), 8, 512)
            for c in range(4):
                jc = j * 4 + c
                pG = pb()
                for kc in range(16):
                    op('pe', 'matmul', inc=(kc == 15), out=pG[:, 0:TT], lhsT=wg[:, kc, c * 128:(c + 1) * 128],
                       rhs=hT3[:, kc, :], start=(kc == 0), stop=(kc == 15))
                pZ = pb()
                for kc in range(8):
                    op('pe', 'matmul', inc=(kc == 7), out=pZ[:, 0:TT], lhsT=wb[:, kc, c * 128:(c + 1) * 128],
                       rhs=yb3[:, kc, :], start=(kc == 0), stop=(kc == 7))
                sg = sgb[jc % 2][:, 0:TT]
                op('act', 'activation', out=sg, in_=pG[:, 0:TT], func=AF.Sigmoid)
                if br == 0:
                    op('dve', 'tensor_tensor', out=macc3[:, jc, :], in0=sg, in1=pZ[:, 0:TT], op=ALU.mult)
                else:
                    op('dve', 'tensor_tensor', out=sg, in0=sg, in1=pZ[:, 0:TT], op=ALU.mult)
                    op('pool', 'tensor_tensor', out=macc3[:, jc, :], in0=macc3[:, jc, :], in1=sg, op=ALU.add)
                yield

    def merge(l, br, defer=True):
        drain()
        pending['gen'] = merge_gen(l, br)
        if not defer:
            drain()

    sgb = [k.sb("sgb%d" % i, [128, 512], F32) for i in range(2)]

    def mixer_ssd(l):
        A = arena.carve([("zs", BF16, NB * 1024), ("xF", BF16, 8 * TT), ("BF", BF16, 4 * TT), ("CF", BF16, 4 * TT),
                         ("xT", BF16, NB * 1024), ("xdt", BF16, NB * 1024), ("BT", BF16, NB * 512),
                         ("dt", F32, NB * 16), ("adt", F32, NB * 16),
                         ("vw", BF16, 1024), ("E0", F32, 512), ("E1", F32, 512), ("MT", BF16, 16 * 128),
                         ("y", F32, 1024), ("sq", F32, 1024), ("yn", BF16, 1024), ("acc0", F32, TT),
                         ("acc1", F32, TT)] + [("cin%d" % j, F32, TT + 3) for j in range(8)])
        cinj = [A["cin%d" % j] for j in range(8)]
        accs = [A['acc0'], A['acc1']]
        zs, xF, BFm, CFm = A['zs'], A['xF'], A['BF'], A['CF']
        xF3 = xF.re("p (c t) -> p c t", t=TT)
        BF3 = BFm.re("p (c t) -> p c t", t=TT)
        CF3 = CFm.re("p (c t) -> p c t", t=TT)
        def zproj(gi):
            wt = ws.next("a_z%d" % gi)
            proj_tm(wt, 512, lambda b, ps, gi=gi: op('act', 'activation',
                                                     out=zs[:, b * 1024 + gi * 512:b * 1024 + gi * 512 + 512], in_=ps,
                                                     func=AF.Silu))

        wt = ws.next("a_dt")

        def ev_dt(b, ps):
            d = A['dt'][:, b * 16:(b + 1) * 16]
            op('dve', 'tensor_tensor', out=d, in0=ps, in1=rowp[l][:, R_DTB:R_DTB + 16], op=ALU.add)
            op('act', 'activation', out=d, in_=d, func=AF.Exp)
            op('act', 'activation', out=d, in_=d, func=AF.Ln, bias=1.0)
            op('dve', 'tensor_tensor', out=A['adt'][:, b * 16:(b + 1) * 16], in0=d, in1=arow[l], op=ALU.mult)
        ck('ssd1')
        proj_tm(wt, 16, ev_dt)
        ck('ssd2')
        for half, names in enumerate((("a_x0", "a_x1"), ("a_B", "a_C"))):
            tl3 = tail_ssd[l].re("p (c t) -> p c t", t=3)
            for gi, nm in enumerate(names):
                wt = ws.next(nm)

                def ev_x(j, ps, gi=gi, half=half):
                    jj = gi * 4 + j
                    ch = half * 8 + jj
                    op('dve', 'tensor_copy', out=cinj[jj][:, 0:3], in_=tl3[:, ch, :])
                    op('act', 'activation', out=cinj[jj][:, 3:3 + TT], in_=ps, func=AF.Copy)
                    op('dve', 'tensor_copy', out=tl3[:, ch, :], in_=cinj[jj][:, TT:TT + 3])
                proj_fm(wt, 512, ev_x)
            for j in range(8):
                ch = half * 8 + j
                acc = accs[j % 2]
                conv_chunk(cinj[j], lambda t, ch=ch: colp[l][:, C_SCW + ch * 4 + t:C_SCW + ch * 4 + t + 1],
                           colp[l][:, C_SCB + ch:C_SCB + ch + 1], acc)
                if half == 0:
                    dstv = xF3[:, j, :]
                else:
                    dstv = BF3[:, j, :] if j < 4 else CF3[:, j - 4, :]
                op('act', 'activation', out=dstv, in_=acc, func=AF.Silu)
            zproj(half)
        ck('ssd3')
        op('act', 'activation', out=stb[:, 0:1024], in_=st_ssd[l], func=AF.Copy)
        dfr = [None]
        for b in (range(NB) if not REV else reversed(range(NB))):
            blk = slice(b * 128, (b + 1) * 128)
            pt = pb().cast(BF16)
            for c in range(8):
                op('pe', 'transpose', inc=(c == 7), out=pt[:, c * 128:(c + 1) * 128], in_=xF3[:, c, blk], identity=ident)
            xTb = A['xT'][:, b * 1024:(b + 1) * 1024]
            op(XTE, 'tensor_copy', out=xTb, in_=pt) if XTE == 'dve' else op('act', 'activation', out=xTb, in_=pt, func=AF.Copy)
            xdtb = A['xdt'][:, b * 1024:(b + 1) * 1024]
            op('dve', 'tensor_tensor', out=xdtb.re("p (h q) -> p h q", q=64), in0=xTb.re("p (h q) -> p h q", q=64),
               in1=A['dt'][:, b * 16:(b + 1) * 16].ubc(2, [128, 16, 64]), op=ALU.mult)
            pt2 = pb().cast(BF16)
            for c in range(4):
                op('pe', 'transpose', inc=(c == 3), out=pt2[:, c * 128:(c + 1) * 128], in_=BF3[:, c, blk], identity=ident)
            BTb = A['BT'][:, b * 512:(b + 1) * 512]
            op('act', 'activation', out=BTb, in_=pt2[:, 0:512], func=AF.Copy)
            ck('ssd4')
            y = A['y']
            lin_attn_chunk(16, 4, 64, A['adt'][:, b * 16:(b + 1) * 16],
                           lambda g: CF3[:, g, blk], lambda g: BF3[:, g, blk],
                           lambda g: BTb[:, g * 128:(g + 1) * 128], xdtb, st_ssd[l], stb[:, 0:1024], y, A)
            ck('ssd5')
            sq = A['sq']
            op('dve', 'tensor_tensor', out=sq.re("p (h q) -> p h q", q=64), in0=xTb.re("p (h q) -> p h q", q=64),
               in1=rowp[l][:, R_DSK:R_DSK + 16].ubc(2, [128, 16, 64]), op=ALU.mult)
            op('dve', 'tensor_tensor', out=y, in0=y, in1=sq, op=ALU.add)
            op('dve', 'tensor_tensor', out=y, in0=y, in1=zs[:, b * 1024:(b + 1) * 1024], op=ALU.mult)
            ck('ssd6')
            if dfr[0] is not None:
                dfr[0]()
            dfr[0] = post_norm_T(l, b, y, 4, None, C_SNRM, A)
            ck('ssd7')
        dfr[0]()

    def mixer_mlstm(l):
        PP = 257
        A = arena.carve([("so", BF16, NB * 1024), ("qF", BF16, 4 * TT), ("kF", BF16, 4 * TT), ("kT", BF16, NB * 512),
                         ("v", BF16, NB * 4 * PP + 2), ("z8", F32, NB * 8), ("eig", F32, NB * 4), ("lf", F32, NB * 4),
                         ("vw", BF16, 4 * PP + 2), ("E0", F32, 512), ("E1", F32, 512), ("MT", BF16, 4 * 128),
                         ("y", F32, 4 * PP + 1), ("hh", F32, 1024), ("sq", F32, 1024), ("yn", BF16, 1024),
                         ("dd", F32, 8)])
        qF3 = A['qF'].re("p (c t) -> p c t", t=TT)
        kF3 = A['kF'].re("p (c t) -> p c t", t=TT)
        wt = ws.next("c_if")

        def ev_if(b, ps):
            z8 = A['z8'][:, b * 8:(b + 1) * 8]
            op('dve', 'tensor_tensor', out=z8, in0=ps, in1=rowp[l][:, R_IFB:R_IFB + 8], op=ALU.add)
            op('act', 'activation', out=A['eig'][:, b * 4:(b + 1) * 4], in_=z8[:, 0:4], func=AF.Exp)
            lf = A['lf'][:, b * 4:(b + 1) * 4]
            op('act', 'activation', out=lf, in_=z8[:, 4:8], func=AF.Exp, scale=-1.0)
            op('act', 'activation', out=lf, in_=lf, func=AF.Ln, bias=1.0)
            op('dve', 'tensor_scalar', out=lf, in0=lf, scalar1=-1.0, scalar2=None, op0=ALU.mult)
        proj_tm(wt, 8, ev_if)
        wt = ws.next("c_q")
        proj_fm(wt, 512, lambda j, ps: op('act', 'activation', out=qF3[:, j, :], in_=ps, func=AF.Copy,
                                          scale=128.0 ** -0.5))
        wt = ws.next("c_k")
        proj_fm(wt, 512, lambda j, ps: op('act', 'activation', out=kF3[:, j, :], in_=ps, func=AF.Copy))
        for gi in range(2):
            wt = ws.next("c_v%d" % gi)

            def ev_v(b, ps, gi=gi):
                vb = A['v'][:, b * 4 * PP:(b + 1) * 4 * PP].re("p (h q) -> p h q", q=PP)
                op('dve', 'tensor_tensor', out=vb[:, gi * 2:gi * 2 + 2, 0:256], in0=ps.re("p (h q) -> p h q", q=256),
                   in1=A['eig'][:, b * 4 + gi * 2:b * 4 + gi * 2 + 2].ubc(2, [128, 2, 256]), op=ALU.mult)
            proj_tm(wt, 512, ev_v)
        for b in range(NB):
            vb = A['v'][:, b * 4 * PP:(b + 1) * 4 * PP].re("p (h q) -> p h q", q=PP)
            op('dve', 'tensor_copy', out=vb[:, :, 256:257], in_=A['eig'][:, b * 4:(b + 1) * 4].ubc(2, [128, 4, 1]))
        for gi in range(2):
            wt = ws.next("c_o%d" % gi)
            proj_tm(wt, 512, lambda b, ps, gi=gi: op('act', 'activation',
                                                     out=A['so'][:, b * 1024 + gi * 512:b * 1024 + gi * 512 + 512],
                                                     in_=ps, func=AF.Sigmoid))
        op('act', 'activation', out=stb[:, 0:4 * PP], in_=st_ml[l], func=AF.Copy)
        dfr = [None]
        for b in range(NB):
            blk = slice(b * 128, (b + 1) * 128)
            pt = pb().cast(BF16)
            for c in range(4):
                op('pe', 'transpose', inc=(c == 3), out=pt[:, c * 128:(c + 1) * 128], in_=kF3[:, c, blk], identity=ident)
            kTb = A['kT'][:, b * 512:(b + 1) * 512]
            op('act', 'activation', out=kTb, in_=pt[:, 0:512], func=AF.Copy)
            y = A['y'][:, 0:4 * PP]
            lin_attn_chunk(4, 4, PP, A['lf'][:, b * 4:(b + 1) * 4],
                           lambda g: qF3[:, g, blk], lambda g: kF3[:, g, blk],
                           lambda g: kTb[:, g * 128:(g + 1) * 128], A['v'][:, b * 4 * PP:(b + 1) * 4 * PP],
                           st_ml[l], stb[:, 0:4 * PP], y, A)
            y3 = y.re("p (h q) -> p h q", q=PP)
            dd = A['dd'][:, 0:4]
            op('act', 'activation', out=dd.ubc(2, [128, 4, 1]) if False else dd, in_=y3[:, :, 256], func=AF.Abs)
            op('dve', 'tensor_scalar', out=dd, in0=dd, scalar1=1.0, scalar2=None, op0=ALU.max)
            op('dve', 'reciprocal', out=dd, in_=dd)
            hh = A['hh']
            op('dve', 'tensor_tensor', out=hh.re("p (h q) -> p h q", q=256), in0=y3[:, :, 0:256],
               in1=dd.ubc(2, [128, 4, 256]), op=ALU.mult)
            if dfr[0] is not None:
                dfr[0]()
            dfr[0] = post_norm_T(l, b, hh, 4, A['so'][:, b * 1024:(b + 1) * 1024], C_MNRM, A)
        dfr[0]()

    def mixer_hgrn(l):
        NCH = TT // 64
        A = arena.carve([("X1", F32, 8 * TT), ("X2", F32, 8 * TT), ("X3", F32, 8 * TT), ("X4", F32, 8 * TT),
                         ("X5", F32, 8 * TT), ("qt", BF16, 8 * TT), ("kt", BF16, 8 * TT),
                         ("vT", BF16, NB * 1024), ("gs", BF16, NB * 1024)])
        A['lam'], A['mu'], A['nu'], A['gst'] = (hgp[:, i * 32:i * 32 + 8 * NCH] for i in range(4))
        X1, X2, X3, X4, X5 = (A['X%d' % i].re("p (h t) -> p h t", t=TT) for i in range(1, 6))
        for gi in range(2):
            wt = ws.next("b_q%d" % gi)
            proj_fm(wt, 512, lambda j, ps, gi=gi: op('act', 'activation', out=X4[:, gi * 4 + j, :], in_=ps, func=AF.Silu))
        for gi in range(2):
            wt = ws.next("b_f%d" % gi)

            def ev_f(j, ps, gi=gi):
                h = gi * 4 + j
                op('act', 'activation', out=X1[:, h, :], in_=ps, func=AF.Sigmoid)
                op('dve', 'tensor_scalar', out=X1[:, h, :], in0=X1[:, h, :], scalar1=lbp[l][:, 8 + h:9 + h],
                   scalar2=lbp[l][:, h:h + 1], op0=ALU.mult, op1=ALU.add)
                op('dve', 'tensor_scalar', out=X2[:, h, :], in0=X1[:, h, :], scalar1=-1.0, scalar2=1.0, op0=ALU.mult,
                   op1=ALU.add)
                op('act', 'activation', out=X1[:, h, :], in_=X1[:, h, :], func=AF.Ln)
                op('dve', 'tensor_tensor_scan', out=X3[:, h, :], data0=ones, data1=X1[:, h, :], initial=0.0,
                   op0=ALU.mult, op1=ALU.add)
            proj_fm(wt, 512, ev_f)
        G4 = A['X3'].re("p (h c j) -> p h c j", c=NCH, j=64)
        Gc = A['X3'].re("p (hc j) -> p hc j", j=64)
        Gd = A['X5'].re("p (hc j) -> p hc j", j=64)
        op('dve', 'tensor_tensor', out=Gd, in0=Gc, in1=Gc[:, :, 31:32].bc([128, 8 * NCH, 64]), op=ALU.subtract)
        gst = A['gst'].re("p (h c) -> p h c", c=NCH)
        lam = A['lam'].re("p (h c) -> p h c", c=NCH)
        mu = A['mu'].re("p (h c) -> p h c", c=NCH)
        nu = A['nu'].re("p (h c) -> p h c", c=NCH)
        op('dve', 'memset', ap=A['gst'], constant=0.0)
        op('dve', 'tensor_copy', out=gst[:, :, 1:NCH], in_=G4[:, :, 0:NCH - 1, 63])
        op('dve', 'tensor_tensor', out=lam, in0=G4[:, :, :, 63], in1=gst, op=ALU.subtract)
        op('dve', 'tensor_tensor', out=mu, in0=G4[:, :, :, 63], in1=G4[:, :, :, 31], op=ALU.subtract)
        op('dve', 'tensor_tensor', out=nu, in0=G4[:, :, :, 31], in1=gst, op=ALU.subtract)
        for t in ('lam', 'mu', 'nu'):
            op('act', 'activation', out=A[t], in_=A[t], func=AF.Exp)
        op('dve', 'tensor_scalar', out=A['X1'], in0=A['X5'], scalar1=80.0, scalar2=None, op0=ALU.min)
        op('act', 'activation', out=A['X1'], in_=A['X1'], func=AF.Exp)
        op('dve', 'scalar_tensor_tensor', out=A['qt'], in0=A['X4'], scalar=128.0 ** -0.5, in1=A['X1'], op0=ALU.mult,
           op1=ALU.mult)
        op('dve', 'tensor_scalar', out=A['X4'], in0=A['X5'], scalar1=-1.0, scalar2=80.0, op0=ALU.mult, op1=ALU.min)
        op('act', 'activation', out=A['X4'], in_=A['X4'], func=AF.Exp)
        op('dve', 'tensor_tensor', out=A['kt'], in0=A['X2'], in1=A['X4'], op=ALU.mult)
        qt3 = A['qt'].re("p (h t) -> p h t", t=TT)
        kt3 = A['kt'].re("p (h t) -> p h t", t=TT)
        for gi in range(2):
            wt = ws.next("b_i%d" % gi)
            proj_tm(wt, 512, lambda b, ps, gi=gi: op('act', 'activation',
                                                     out=A['vT'][:, b * 1024 + gi * 512:b * 1024 + gi * 512 + 512],
                                                     in_=ps, func=AF.Copy))
        for gi in range(2):
            wt = ws.next("b_g%d" % gi)
            proj_tm(wt, 512, lambda b, ps, gi=gi: op('act', 'activation',
                                                     out=A['gs'][:, b * 1024 + gi * 512:b * 1024 + gi * 512 + 512],
                                                     in_=ps, func=AF.Silu))
        A2 = A
        qt3 = A2['qt'].re("p (h t) -> p h t", t=TT)
        kt3 = A2['kt'].re("p (h t) -> p h t", t=TT)
        lam = A2['lam'].re("p (h c) -> p h c", c=NCH)
        mu = A2['mu'].re("p (h c) -> p h c", c=NCH)
        nu = A2['nu'].re("p (h c) -> p h c", c=NCH)
        vT, gsv = A2['vT'], A2['gs']
        qa = A2['X1'].cast(BF16)[:, 0:8 * TT]
        qb = A2['X1'].cast(BF16)[:, 8 * TT:16 * TT]
        qa3 = qa.re("p (h t) -> p h t", t=TT)
        qb3 = qb.re("p (h t) -> p h t", t=TT)
        op('dve', 'tensor_tensor', out=qa3, in0=qt3, in1=evn.ubc(1, [128, 8, TT]), op=ALU.mult)
        op('dve', 'tensor_tensor', out=qb3, in0=qt3, in1=odd.ubc(1, [128, 8, TT]), op=ALU.mult)
        kTt = A2['X2'].cast(BF16)[:, 0:NB * 1024]
        At = A2['X2'].cast(BF16)[:, NB * 1024:2 * NB * 1024]
        ob = A2['X3'][:, 0:1024]
        AA = {'sq': A2['X3'][:, 1024:2048], 'yn': A2['X4'].cast(BF16)[:, 0:1024]}
        tmpu = A2['X5'][:, 0:1024]
        S = st_hg[l]
        S3 = S.re("p (h e) -> p h e", e=128)
        sb03 = sb0.re("p (h e) -> p h e", e=128)
        sb13 = sb1.re("p (h e) -> p h e", e=128)
        tmpu3 = tmpu.re("p (h e) -> p h e", e=128)
        dfr = [None]
        for b in range(NB):
            blk = slice(b * 128, (b + 1) * 128)
            pt = pb().cast(BF16)
            for h in range(8):
                op('pe', 'transpose', inc=(h == 7), out=pt[:, h * 128:(h + 1) * 128], in_=kt3[:, h, blk], identity=ident)
            kTb = kTt[:, b * 1024:(b + 1) * 1024]
            op('act', 'activation', out=kTb, in_=pt, func=AF.Copy)
            Atb = At[:, b * 1024:(b + 1) * 1024]
            At3 = Atb.re("p (h i) -> p h i", i=128)
            for hb in range(2):
                pa = pb()
                for i in range(4):
                    h = hb * 4 + i
                    op('pe', 'matmul', inc=(i == 3), out=pa[:, i * 128:(i + 1) * 128], lhsT=kt3[:, h, blk],
                       rhs=qt3[:, h, blk], start=True, stop=True)
                op('dve', 'tensor_tensor', out=At3[:, hb * 4:hb * 4 + 4, :], in0=pa.re("p (h i) -> p h i", i=128),
                   in1=hmask.ubc(1, [128, 4, 128]), op=ALU.mult)
                filler(1)
            vTb = vT[:, b * 1024:(b + 1) * 1024]
            for ci, sbx3 in ((0, sb03), (1, sb13)):
                c = 2 * b + ci
                rows = slice(ci * 64, ci * 64 + 64)
                op('dve', 'tensor_tensor', out=sbx3, in0=S3, in1=nu[:, :, c:c + 1].bc([128, 8, 128]), op=ALU.mult)
                for hb in range(2):
                    pu = pb()
                    for i in range(4):
                        h = hb * 4 + i
                        op('pe', 'matmul', inc=(i == 3), out=pu[:, i * 128:(i + 1) * 128],
                           lhsT=kTb[rows, h * 128:(h + 1) * 128], rhs=vTb[rows, h * 128:(h + 1) * 128], start=True,
                           stop=True)
                    hs = slice(hb * 4, hb * 4 + 4)
                    op('dve', 'tensor_tensor', out=tmpu3[:, hs, :], in0=pu.re("p (h e) -> p h e", e=128),
                       in1=mu[:, hs, c:c + 1].bc([128, 4, 128]), op=ALU.mult)
                    op('dve', 'tensor_tensor', out=S3[:, hs, :], in0=S3[:, hs, :],
                       in1=lam[:, hs, c:c + 1].bc([128, 4, 128]), op=ALU.mult)
                    op('dve', 'tensor_tensor', out=S3[:, hs, :], in0=S3[:, hs, :], in1=tmpu3[:, hs, :], op=ALU.add)
                    filler(1)
            for hb in range(2):
                po = pb()
                for i in range(4):
                    h = hb * 4 + i
                    o = po[:, i * 128:(i + 1) * 128]
                    op('pe', 'matmul', inc=False, out=o, lhsT=At3[:, h, :], rhs=vTb[:, h * 128:(h + 1) * 128],
                       start=True, stop=False)
                    op('pe', 'matmul', inc=False, out=o, lhsT=qa3[:, h, blk], rhs=sb03[:, h, :], start=False,
                       stop=False)
                    op('pe', 'matmul', inc=(i == 3), out=o, lhsT=qb3[:, h, blk], rhs=sb13[:, h, :], start=False,
                       stop=True)
                op('act', 'activation', out=ob[:, hb * 512:(hb + 1) * 512], in_=po, func=AF.Copy)
                filler(1)
            if dfr[0] is not None:
                dfr[0]()
            dfr[0] = post_norm_T(l, b, ob, 8, gsv[:, b * 1024:(b + 1) * 1024], C_HNRM, AA)
        dfr[0]()

    def mixer_lru(l):
        A = arena.carve([("xc", F32, 8 * TT), ("xcb", BF16, 8 * TT), ("hF", F32, 8 * TT)]
                        + [(nm + str(i), F32, TT) for i in range(2) for nm in ("r", "ig", "a", "a2", "u")]
                        + [("gg0", F32, TT), ("gg1", F32, TT)] + [("cin%d" % j, F32, TT + 3) for j in range(8)])
        cinj = [A["cin%d" % j] for j in range(8)]
        xc3 = A['xc'].re("p (c t) -> p c t", t=TT)
        xcb3 = A['xcb'].re("p (c t) -> p c t", t=TT)
        hF3 = A['hF'].re("p (c t) -> p c t", t=TT)
        lw = lruw[l].re("p (m n d) -> p m n d", m=2, n=8)
        tl3 = tail_lru[l].re("p (c t) -> p c t", t=3)
        for gi in range(2):
            wt = ws.next("d_x%d" % gi)

            def ev_x(j, ps, gi=gi):
                n = gi * 4 + j
                op('dve', 'tensor_copy', out=cinj[n][:, 0:3], in_=tl3[:, n, :])
                op('act', 'activation', out=cinj[n][:, 3:3 + TT], in_=ps, func=AF.Copy)
                op('dve', 'tensor_copy', out=tl3[:, n, :], in_=cinj[n][:, TT:TT + 3])
            proj_fm(wt, 512, ev_x)
        for n in range(8):
            conv_chunk(cinj[n], lambda t, n=n: colp[l][:, C_LCW + n * 4 + t:C_LCW + n * 4 + t + 1],
                       colp[l][:, C_LCB + n:C_LCB + n + 1], xc3[:, n, :])
            op('act', 'activation', out=xcb3[:, n, :], in_=xc3[:, n, :], func=AF.Copy)
            filler(1)
        for n in range(8):
            T = lambda nm, n=n: A[nm + str(n % 2)]
            pr = pb()
            op('pe', 'matmul', inc=False, out=pr[:, 0:TT], lhsT=lw[:, 0, n, :], rhs=xcb3[:, n, :], start=True, stop=True)
            op('pe', 'matmul', out=pr[:, TT:2 * TT], lhsT=lw[:, 1, n, :], rhs=xcb3[:, n, :], start=True, stop=True)
            op('act', 'activation', out=T('r'), in_=pr[:, 0:TT], func=AF.Sigmoid, bias=colp[l][:, C_LBA + n:C_LBA + n + 1])
            op('act', 'activation', out=T('ig'), in_=pr[:, TT:2 * TT], func=AF.Sigmoid,
               bias=colp[l][:, C_LBI + n:C_LBI + n + 1])
            op('act', 'activation', out=T('a'), in_=T('r'), func=AF.Exp, scale=lrup[l][:, n:n + 1])
            op('act', 'activation', out=T('a2'), in_=T('r'), func=AF.Exp, scale=lrup[l][:, 8 + n:9 + n])
            op('act', 'activation', out=T('a2'), in_=T('a2'), func=AF.Sqrt, scale=-1.0, bias=1.0)
            op('dve', 'tensor_tensor', out=T('u'), in0=xc3[:, n, :], in1=T('ig'), op=ALU.mult)
            op('dve', 'tensor_tensor', out=T('u'), in0=T('u'), in1=T('a2'), op=ALU.mult)
            op('dve', 'tensor_tensor_scan', out=hF3[:, n, :], data0=T('a'), data1=T('u'), initial=st_lru[l][:, n:n + 1],
               op0=ALU.mult, op1=ALU.add)
            op('dve', 'tensor_copy', out=st_lru[l][:, n:n + 1], in_=hF3[:, n, TT - 1:TT])
            filler(1)
        drain()
        for gi in range(2):
            wt = ws.next("d_g%d" % gi)

            def ev_g(j, ps, gi=gi):
                n = gi * 4 + j
                gg = A['gg%d' % (n % 2)]
                op('act', 'activation', out=gg, in_=ps, func=AF.Gelu_apprx_tanh)
                op('dve', 'tensor_tensor', out=ybr3s[cur['br'] % 2][:, n, :], in0=hF3[:, n, :], in1=gg, op=ALU.mult)
            proj_fm(wt, 512, ev_g)

    try:
      for t in range(ntiles):
          t0 = t * TT
          for b in range(NB):
              k.dma('sp', xs[b], x_d[t0 + b * 128:t0 + (b + 1) * 128, :])
          for l in range(nlayers):
              dbg = taps and t == 0 and l == 0
              rmsnorm_hT(l, C_MIXN)
              if dbg:
                  tap("hT", hT, [128, 16 * TT], BF16)
              ck('norm')
              cur['br'] = 0
              mixer_ssd(l)
              if dbg:
                  tap("ya", ybrs[cur['br'] % 2], [128, 8 * TT], BF16)
              ck('ssd')
              merge(l, 0)
              ck('merge0')
              cur['br'] = 1
              mixer_hgrn(l)
              drain()
              if dbg:
                  tap("yb", ybrs[cur['br'] % 2], [128, 8 * TT], BF16)
              ck('hgrn')
              merge(l, 1)
              cur['br'] = 2
              mixer_mlstm(l)
              drain()
              if dbg:
                  tap("yc", ybrs[cur['br'] % 2], [128, 8 * TT], BF16)
              ck('mlstm')
              merge(l, 2)
              cur['br'] = 3
              mixer_lru(l)
              if dbg:
                  tap("yd", ybrs[cur['br'] % 2], [128, 8 * TT], BF16)
              ck('lru')
              merge(l, 3, defer=False)
              if dbg:
                  tap("macc", macc, [128, 16 * TT])
              op('act', 'activation', out=hT, in_=macc, func=AF.Copy)
              for j in range(4):
                  w = w3(ws.next("wout_%d" % j), 16, 512)
                  for b in range(NB):
                      ps = pb()
                      for kc in range(16):
                          op('pe', 'matmul', inc=(kc == 15), out=ps, lhsT=hT3[:, kc, b * 128:(b + 1) * 128], rhs=w[:, kc, :],
                             start=(kc == 0), stop=(kc == 15))
                      xv = xs[b][:, j * 512:(j + 1) * 512]
                      op('dve', 'tensor_tensor', out=xv, in0=xv, in1=ps, op=ALU.add)
              if dbg:
                  tap("x1", xs[0], [128, D])
              ck('wout')
              rmsnorm_hT(l, C_MLPN)
              for q in range(4):
                  for j in range(4):
                      wt = ws.next("wup_%d" % (q * 4 + j))

                      def ev_up(c, ps, j=j):
                          r = sgb[c % 2][:, 0:TT]
                          op('act', 'activation', out=r, in_=ps, func=AF.Relu)
                          op('dve', 'tensor_tensor', out=aq3[:, j * 4 + c, :], in0=r, in1=r, op=ALU.mult)
                      proj_fm(wt, 512, ev_up)
                  for j in range(4):
                      w = w3(ws.next("wdn_%d_%d" % (q, j)), 16, 512)
                      for b in range(NB):
                          ps = pb()
                          for kc in range(16):
                              op('pe', 'matmul', inc=(kc == 15), out=ps, lhsT=aq3[:, kc, b * 128:(b + 1) * 128],
                                 rhs=w[:, kc, :], start=(kc == 0), stop=(kc == 15))
                          xv = xs[b][:, j * 512:(j + 1) * 512]
                          op('dve', 'tensor_tensor', out=xv, in0=xv, in1=ps, op=ALU.add)
              if dbg:
                  tap("x2", xs[0], [128, D])
              ck('mlp')
              rmsnorm_hT(l, C_PLEN)
              A = arena.carve([("pst", F32, 2 * TT), ("pb16", BF16, 2 * TT)])
              k.dma('sp', A['pst'].re("p (c t) -> p c t", t=TT),
                    pT_d[l].re("(c p) t -> p c t", p=128)[:, :, t0:t0 + TT])
              op('act', 'activation', out=A['pb16'], in_=A['pst'], func=AF.Copy)
              p3 = A['pb16'].re("p (c t) -> p c t", t=TT)
              for j in range(4):
                  wg = w3(ws.next("wpg_%d" % j), 16, 512)
                  wp = w3(ws.next("wple_%d" % j, keep_prev=True), 2, 512)
                  for b in range(NB):
                      pG = pb()
                      for kc in range(16):
                          op('pe', 'matmul', inc=(kc == 15), out=pG, lhsT=hT3[:, kc, b * 128:(b + 1) * 128], rhs=wg[:, kc, :],
                             start=(kc == 0), stop=(kc == 15))
                      pP = pb()
                      for kc in range(2):
                          op('pe', 'matmul', inc=(kc == 1), out=pP, lhsT=p3[:, kc, b * 128:(b + 1) * 128], rhs=wp[:, kc, :],
                             start=(kc == 0), stop=(kc == 1))
                      sg = sgb[(j * NB + b) % 2]
                      op('act', 'activation', out=sg, in_=pG, func=AF.Sigmoid)
                      op('dve', 'tensor_tensor', out=sg, in0=sg, in1=pP, op=ALU.mult)
                      xv = xs[b][:, j * 512:(j + 1) * 512]
                      op('pool', 'tensor_tensor', out=xv, in0=xv, in1=sg, op=ALU.add)
          A = arena.carve([("fn", F32, D), ("ob0", F32, D), ("ob1", F32, D)] + [("xn%d" % b, BF16, D) for b in range(NB)])
          xn = [A["xn%d" % b] for b in range(NB)]
          k.dma('sp', A['fn'], fnorm_d.m(lambda a: a.partition_broadcast(128)))
          for b in range(NB):
              ob = A['ob%d' % b]
              op('act', 'activation', out=xn[b], in_=xs[b], func=AF.Square, accum_out=ss[:, b:b + 1])
              op('act', 'activation', out=ss[:, 4 + b:5 + b], in_=ss[:, b:b + 1], func=AF.Sqrt, scale=1.0 / D, bias=EPS)
              op('dve', 'reciprocal', out=ss[:, 4 + b:5 + b], in_=ss[:, 4 + b:5 + b])
              op('dve', 'scalar_tensor_tensor', out=ob, in0=xs[b], scalar=ss[:, 4 + b:5 + b], in1=A['fn'], op0=ALU.mult,
                 op1=ALU.mult)
              k.dma('sp', out_d[t0 + b * 128:t0 + (b + 1) * 128, :], ob)
    except _Stop:
        pass
    k.finish()
    return nc, k


def _pack(W, groups, kc):
    parts = []
    for c0, n in groups:
        blk = W[:, c0:c0 + n].reshape(kc, 128, n).transpose(1, 0, 2).reshape(128, kc * n)
        parts.append(blk)
    return np.ascontiguousarray(np.concatenate(parts, axis=1))


def _col(v):
    return v.reshape(-1, 128).T


def prepare_weights(inp):
    L = 2
    f = lambda a: np.asarray(a, dtype=np.float32)
    g512 = lambda n: [(j * 512, 512) for j in range(n // 512)]
    win = np.stack([_pack(f(inp['w_in'][l]), [(c0, n) for _, c0, n in WIN_GROUPS], 16) for l in range(L)])
    wbr = np.stack([np.concatenate([_pack(f(inp['w_branch'][l, br]), g512(2048), 8) for br in range(4)], axis=1)
                    for l in range(L)])
    wout = np.stack([_pack(f(inp['w_out'][l]), g512(2048), 16) for l in range(L)])
    wup = np.stack([_pack(f(inp['w_up'][l]), g512(DFF), 16) for l in range(L)])
    wdn = np.stack([np.concatenate([_pack(f(inp['w_down'][l])[q * 2048:(q + 1) * 2048], g512(2048), 16)
                                    for q in range(4)], axis=1) for l in range(L)])
    wple = np.stack([_pack(f(inp['w_ple'][l]), g512(2048), 2) for l in range(L)])
    wpg = np.stack([_pack(f(inp['w_ple_gate'][l]), g512(2048), 16) for l in range(L)])
    lruw = np.stack([np.concatenate([f(inp['lru_wa'][l]).transpose(1, 0, 2).reshape(128, 1024),
                                     f(inp['lru_wi'][l]).transpose(1, 0, 2).reshape(128, 1024)], axis=1)
                     for l in range(L)])
    colp = np.zeros((L, 128, NCOL), np.float32)
    rowp = np.zeros((L, 1, NROW), np.float32)
    for l in range(L):
        c = colp[l]
        c[:, C_MIXN:C_MIXN + 16] = _col(f(inp['mix_norm'][l]))
        c[:, C_MLPN:C_MLPN + 16] = _col(f(inp['mlp_norm'][l]))
        c[:, C_PLEN:C_PLEN + 16] = _col(f(inp['ple_norm'][l]))
        c[:, C_SCW:C_SCW + 64] = f(inp['ssm_conv_w'][l]).T.reshape(16, 128, 4).transpose(1, 0, 2).reshape(128, 64)
        c[:, C_SCB:C_SCB + 16] = _col(f(inp['ssm_conv_b'][l]))
        c[:, C_SNRM:C_SNRM + 8] = _col(f(inp['ssm_norm'][l]))
        c[:, C_HNRM:C_HNRM + 8] = _col(f(inp['hgrn_norm'][l]))
        c[:, C_MNRM:C_MNRM + 8] = _col(f(inp['mlstm_norm'][l]))
        c[:, C_LCW:C_LCW + 32] = f(inp['lru_conv_w'][l]).T.reshape(8, 128, 4).transpose(1, 0, 2).reshape(128, 32)
        c[:, C_LCB:C_LCB + 8] = _col(f(inp['lru_conv_b'][l]))
        c[:, C_LBA:C_LBA + 8] = _col(f(inp['lru_ba'][l]))
        c[:, C_LBI:C_LBI + 8] = _col(f(inp['lru_bi'][l]))
        c[:, C_LAP:C_LAP + 8] = _col(f(inp['lru_a_param'][l]))
        c[:, C_LB0:C_LB0 + 8] = _col(f(inp['hgrn_lb_logits'][0]))
        c[:, C_LB1:C_LB1 + 8] = _col(f(inp['hgrn_lb_logits'][1]))
        r = rowp[l, 0]
        r[R_DTB:R_DTB + 16] = f(inp['ssm_dt_bias'][l])
        r[R_ALOG:R_ALOG + 16] = f(inp['ssm_a_log'][l])
        r[R_DSK:R_DSK + 16] = f(inp['ssm_d'][l])
        r[R_IFB:R_IFB + 4] = f(inp['mlstm_i_bias'][l])
        r[R_IFB + 4:R_IFB + 8] = f(inp['mlstm_f_bias'][l])
    return dict(win=win, wbr=wbr, wout=wout, wup=wup, wdn=wdn, wple=wple, wpg=wpg, lruw=np.ascontiguousarray(lruw),
                colp=colp, rowp=rowp, fnorm=f(inp['final_norm']).reshape(1, D))


def kernel(**inputs):
    x = np.asarray(inputs['x'], dtype=np.float32)
    p = np.asarray(inputs['p'], dtype=np.float32)
    wts = prepare_weights(inputs)
    nc, _ = build()
    in_maps = []
    for b in range(8):
        m = dict(wts)
        m['x'] = np.ascontiguousarray(x[b])
        m['pT'] = np.ascontiguousarray(p[:, b].transpose(0, 2, 1))
        in_maps.append(m)
    res = run_bass_kernel_spmd(nc, in_maps, core_ids=list(range(8)))
    return np.stack([r['out'] for r in res.results], axis=0)
```
